# Optimizing a Trainium2 kernel written in Bass

```python
import jax, jax.numpy as jnp
from jax import lax
import numpy as np

D_MODEL = 1024
BATCH = 4
SEQ = 4096
DEPTH = 4

HEAD_DIM = 64
N_BRANCH = 4
BRANCH_WIDTH = D_MODEL // N_BRANCH
NSA_HEADS = BRANCH_WIDTH // HEAD_DIM
FOX_HEADS = BRANCH_WIDTH // HEAD_DIM
GMLP_GROUPS = BRANCH_WIDTH // HEAD_DIM
CONV_CH = BRANCH_WIDTH
ROPE_THETA = 500000.0
ROPE_DIM = HEAD_DIM // 4
Q_BLOCK = 128
CMP_LEN = 32
CMP_STRIDE = 16
SEL_LEN = 64
SEL_TOPK = 16
N_LOCAL_BLOCKS = 2
WINDOW = 512
GMLP_CHUNK = 128
CONV_WIDTH = 31
N_EXPERTS = 32
TOP_K = 4
D_FF = D_MODEL
SWIGLU_LIMIT = 7.0
SWIGLU_ALPHA = 1.702
MOE_BLOCK = 128
LN_EPS = 1e-5
DEEPNORM_ALPHA = (2 * DEPTH) ** 0.25
DEEPNORM_BETA = (8 * DEPTH) ** -0.25
MASK_VALUE = -1e30
FORCED_SCORE = 1e9
SPLIT_SIZES = (NSA_HEADS * HEAD_DIM, HEAD_DIM, HEAD_DIM, HEAD_DIM, HEAD_DIM, HEAD_DIM, HEAD_DIM, 3 * NSA_HEADS,
               FOX_HEADS * HEAD_DIM, FOX_HEADS * HEAD_DIM, FOX_HEADS * HEAD_DIM, FOX_HEADS,
               BRANCH_WIDTH, BRANCH_WIDTH,
               CONV_CH, CONV_CH,
               N_BRANCH * D_MODEL)
N_IN = sum(SPLIT_SIZES)

kernel_name = 'hybrid_nsa_fox_gmlp_conv_moe'


def layer_norm(x, g, b):
    xf = x.astype(jnp.float32)
    mu = jnp.mean(xf, axis=-1, keepdims=True)
    var = jnp.mean(jnp.square(xf - mu), axis=-1, keepdims=True)
    return ((xf - mu) * lax.rsqrt(var + LN_EPS) * g + b).astype(x.dtype)


def masked_softmax(logits, mask):
    p = jax.nn.softmax(jnp.where(mask, logits.astype(jnp.float32), MASK_VALUE), axis=-1)
    return jnp.where(mask, p, 0.0)


def partial_rope(t, positions):
    half = ROPE_DIM // 2
    inv_freq = ROPE_THETA ** (-jnp.arange(half, dtype=jnp.float32) / half)
    ang = positions.astype(jnp.float32)[:, :, None, None] * inv_freq
    cos, sin = jnp.cos(ang), jnp.sin(ang)
    tr = t[..., :ROPE_DIM].astype(jnp.float32)
    t1, t2 = tr[..., :half], tr[..., half:]
    rot = jnp.concatenate([t1 * cos - t2 * sin, t2 * cos + t1 * sin], axis=-1).astype(t.dtype)
    return jnp.concatenate([rot, t[..., ROPE_DIM:]], axis=-1)


def nsa_mixer(q, k_cmp, v_cmp, k_sel, v_sel, k_win, v_win, gate_logits,
              pe_k, pe_v, w1_k, w2_k, w1_v, w2_v):
    B, S, H, Dh = q.shape
    scale = Dh ** -0.5
    n_cmp = (S - CMP_LEN) // CMP_STRIDE + 1
    cmp_idx = np.arange(n_cmp)[:, None] * CMP_STRIDE + np.arange(CMP_LEN)[None, :]
    cmp_end = jnp.asarray(cmp_idx[:, -1], jnp.int32)

    def compress(t, pe, w1, w2):
        blocks = (t[:, cmp_idx] + pe).reshape(B, n_cmp, CMP_LEN * Dh)
        return jax.nn.gelu(blocks @ w1) @ w2

    kc = compress(k_cmp, pe_k, w1_k, w2_k)
    vc = compress(v_cmp, pe_v, w1_v, w2_v)
    n_sel = S // SEL_LEN
    top_k = min(SEL_TOPK, n_sel)
    sel_start = np.arange(n_sel) * SEL_LEN
    cmp_start = cmp_idx[:, 0]
    overlap = jnp.asarray(((cmp_start[:, None] <= sel_start[None, :] + SEL_LEN - 1)
                           & (cmp_start[:, None] + CMP_LEN - 1 >= sel_start[None, :])).astype(np.float32))
    k_blocks = k_sel.reshape(B, n_sel, SEL_LEN, Dh)
    v_blocks = v_sel.reshape(B, n_sel, SEL_LEN, Dh)
    k_pad = jnp.pad(k_win, ((0, 0), (WINDOW, 0), (0, 0)))
    v_pad = jnp.pad(v_win, ((0, 0), (WINDOW, 0), (0, 0)))
    gates = jax.nn.sigmoid(gate_logits.astype(jnp.float32)).reshape(B, S, H, 3).astype(q.dtype)
    blk_ids = jnp.arange(n_sel)

    def block(i):
        t0 = i * Q_BLOCK
        tq = t0 + jnp.arange(Q_BLOCK)
        qb = lax.dynamic_slice_in_dim(q, t0, Q_BLOCK, axis=1)
        s_c = jnp.einsum('bqhd,bcd->bhqc', qb, kc) * scale
        p_c = masked_softmax(s_c, cmp_end[None, :] <= tq[:, None])
        o_c = jnp.einsum('bhqc,bcd->bqhd', p_c.astype(vc.dtype), vc)
        imp = jnp.einsum('bhqc,cj->bqj', p_c, overlap)
        cur = (tq // SEL_LEN)[:, None]
        causal = blk_ids[None, :] <= cur
        forced = (blk_ids[None, :] == 0) | (causal & (blk_ids[None, :] > cur - N_LOCAL_BLOCKS))
        imp = jnp.where(forced, FORCED_SCORE, jnp.where(causal, imp, -1.0))
        top_val, top_idx = lax.top_k(imp, top_k)
        ks = jax.vmap(lambda kb, ix: kb[ix])(k_blocks, top_idx).reshape(B, Q_BLOCK, top_k * SEL_LEN, Dh)
        vs = jax.vmap(lambda vb, ix: vb[ix])(v_blocks, top_idx).reshape(B, Q_BLOCK, top_k * SEL_LEN, Dh)
        key_pos = (top_idx[..., None] * SEL_LEN + jnp.arange(SEL_LEN)).reshape(B, Q_BLOCK, top_k * SEL_LEN)
        valid = jnp.repeat(top_val >= 0.0, SEL_LEN, axis=-1) & (key_pos <= tq[None, :, None])
        s_s = jnp.einsum('bqhd,bqnd->bqhn', qb, ks) * scale
        p_s = masked_softmax(s_s, valid[:, :, None, :])
        o_s = jnp.einsum('bqhn,bqnd->bqhd', p_s.astype(vs.dtype), vs)
        kw = lax.dynamic_slice_in_dim(k_pad, t0, Q_BLOCK + WINDOW, axis=1)
        vw = lax.dynamic_slice_in_dim(v_pad, t0, Q_BLOCK + WINDOW, axis=1)
        kpos = t0 - WINDOW + jnp.arange(Q_BLOCK + WINDOW)
        diff = tq[:, None] - kpos[None, :]
        in_win = (diff >= 0) & (diff < WINDOW) & (kpos[None, :] >= 0)
        s_w = jnp.einsum('bqhd,bkd->bhqk', qb, kw) * scale
        p_w = masked_softmax(s_w, in_win)
        o_w = jnp.einsum('bhqk,bkd->bqhd', p_w.astype(vw.dtype), vw)
        g = lax.dynamic_slice_in_dim(gates, t0, Q_BLOCK, axis=1)
        return g[..., 0:1] * o_c + g[..., 1:2] * o_s + g[..., 2:3] * o_w

    out = lax.map(block, jnp.arange(S // Q_BLOCK))
    return out.transpose(1, 0, 2, 3, 4).reshape(B, S, H * Dh)


def fox_mixer(q, k, v, f_logits):
    B, S, H, Dh = q.shape
    scale = Dh ** -0.5
    log_f = jax.nn.log_sigmoid(f_logits.astype(jnp.float32))
    cum = lax.cumsum(log_f, axis=1).transpose(0, 2, 1)
    kpos = jnp.arange(S)

    def block(i):
        t0 = i * Q_BLOCK
        tq = t0 + jnp.arange(Q_BLOCK)
        qb = lax.dynamic_slice_in_dim(q, t0, Q_BLOCK, axis=1)
        cq = lax.dynamic_slice_in_dim(cum, t0, Q_BLOCK, axis=2)
        s = (jnp.einsum('bqhd,bkhd->bhqk', qb, k).astype(jnp.float32) * scale
             + (cq[..., None] - cum[:, :, None, :]))
        p = masked_softmax(s, kpos[None, :] <= tq[:, None])
        return jnp.einsum('bhqk,bkhd->bqhd', p.astype(v.dtype), v)

    out = lax.map(block, jnp.arange(S // Q_BLOCK))
    return out.transpose(1, 0, 2, 3, 4).reshape(B, S, H * Dh)


def gmlp_mixer(u, v, w_s, b_s, ln_g, ln_b):
    B, S, C = v.shape
    u = jax.nn.gelu(u)
    v = layer_norm(jax.nn.gelu(v), ln_g, ln_b)
    n_chunks = S // GMLP_CHUNK
    vr = v.reshape(B, n_chunks, GMLP_CHUNK, GMLP_GROUPS, C // GMLP_GROUPS)
    causal = jnp.tril(jnp.ones((GMLP_CHUNK, GMLP_CHUNK), dtype=bool))
    w = jnp.where(causal[None], w_s, 0.0)
    mixed = jnp.einsum('gts,bcsgd->bctgd', w, vr) + b_s.T[None, None, :, :, None]
    return u * mixed.reshape(B, S, C)


def conv_mixer(a, b_gate, w_dw, b_dw, ln_g, ln_b):
    h = a * jax.nn.sigmoid(b_gate)
    h = lax.conv_general_dilated(h, w_dw, window_strides=(1,), padding=((CONV_WIDTH - 1, 0),),
                                 dimension_numbers=('NWC', 'WIO', 'NWC'),
                                 feature_group_count=h.shape[-1]) + b_dw
    return jax.nn.silu(layer_norm(h, ln_g, ln_b))


def moe_ffn(x, w_router, b_router, w1, b1, w2, b2):
    B, S, D = x.shape
    T = B * S
    xf = x.reshape(T, D)
    logits = (xf @ w_router + b_router).astype(jnp.float32)
    top_val, top_idx = lax.top_k(logits, TOP_K)
    wts = jax.nn.softmax(top_val, axis=-1)
    e_flat = top_idx.reshape(-1)
    tok_flat = jnp.repeat(jnp.arange(T, dtype=jnp.int32), TOP_K)
    w_flat = wts.reshape(-1)
    order = jnp.argsort(e_flat)
    e_sorted = e_flat[order]
    counts = jnp.bincount(e_flat, length=N_EXPERTS)
    padded = ((counts + MOE_BLOCK - 1) // MOE_BLOCK) * MOE_BLOCK
    start = jnp.cumsum(counts) - counts
    pend = jnp.cumsum(padded)
    pstart = pend - padded
    dest = pstart[e_sorted] + (jnp.arange(T * TOP_K) - start[e_sorted])
    n_blocks = -(-(T * TOP_K) // MOE_BLOCK) + N_EXPERTS
    P = n_blocks * MOE_BLOCK
    row_tok = jnp.full((P,), T, jnp.int32).at[dest].set(tok_flat[order])
    row_w = jnp.zeros((P,), jnp.float32).at[dest].set(w_flat[order])
    blk_e = jnp.minimum(jnp.searchsorted(pend, jnp.arange(n_blocks) * MOE_BLOCK, side='right'), N_EXPERTS - 1)
    x_pad = jnp.concatenate([xf, jnp.zeros((1, D), xf.dtype)], axis=0)
    xr = x_pad[row_tok].reshape(n_blocks, MOE_BLOCK, D)

    def expert_block(args):
        xb, e = args
        h = xb @ w1[e] + b1[e]
        glu = jnp.minimum(h[:, 0::2], SWIGLU_LIMIT)
        lin = jnp.clip(h[:, 1::2], -SWIGLU_LIMIT, SWIGLU_LIMIT)
        act = glu * jax.nn.sigmoid(SWIGLU_ALPHA * glu) * (lin + 1.0)
        return act @ w2[e] + b2[e]

    yr = lax.map(expert_block, (xr, blk_e)).reshape(P, D)
    y = jax.ops.segment_sum(yr * row_w[:, None], row_tok, num_segments=T + 1)[:T]
    return y.reshape(B, S, D).astype(x.dtype)


def setup_inputs(seed: int = 0) -> dict:
    key = jax.random.key(seed)
    ks = jax.random.split(key, 30)

    def nrm(k, shape, scale):
        return jax.random.normal(k, shape, jnp.float32) * scale

    def gain(k, shape):
        return 1.0 + 0.1 * jax.random.normal(k, shape, jnp.float32)

    Dh = HEAD_DIM
    return {
        'x': nrm(ks[0], (BATCH, SEQ, D_MODEL), 1.0),
        'positions': jnp.arange(SEQ, dtype=jnp.int32)[None, :]
                     + jax.random.randint(ks[1], (BATCH, 1), 0, 1024, jnp.int32),
        'w_in': nrm(ks[2], (DEPTH, D_MODEL, N_IN), D_MODEL ** -0.5),
        'b_in': nrm(ks[3], (DEPTH, N_IN), 0.02),
        'nsa_pe_k': nrm(ks[4], (DEPTH, CMP_LEN, Dh), 0.1),
        'nsa_pe_v': nrm(ks[5], (DEPTH, CMP_LEN, Dh), 0.1),
        'nsa_cmp_w1_k': nrm(ks[6], (DEPTH, CMP_LEN * Dh, Dh), (CMP_LEN * Dh) ** -0.5),
        'nsa_cmp_w2_k': nrm(ks[7], (DEPTH, Dh, Dh), Dh ** -0.5),
        'nsa_cmp_w1_v': nrm(ks[8], (DEPTH, CMP_LEN * Dh, Dh), (CMP_LEN * Dh) ** -0.5),
        'nsa_cmp_w2_v': nrm(ks[9], (DEPTH, Dh, Dh), Dh ** -0.5),
        'gmlp_ln_g': gain(ks[10], (DEPTH, BRANCH_WIDTH)),
        'gmlp_ln_b': nrm(ks[11], (DEPTH, BRANCH_WIDTH), 0.02),
        'gmlp_w_s': nrm(ks[12], (DEPTH, GMLP_GROUPS, GMLP_CHUNK, GMLP_CHUNK), GMLP_CHUNK ** -0.5),
        'gmlp_b_s': gain(ks[13], (DEPTH, GMLP_GROUPS, GMLP_CHUNK)),
        'conv_w': nrm(ks[14], (DEPTH, CONV_WIDTH, 1, CONV_CH), CONV_WIDTH ** -0.5),
        'conv_b': nrm(ks[15], (DEPTH, CONV_CH), 0.02),
        'conv_ln_g': gain(ks[16], (DEPTH, CONV_CH)),
        'conv_ln_b': nrm(ks[17], (DEPTH, CONV_CH), 0.02),
        'w_br': nrm(ks[18], (DEPTH, N_BRANCH, BRANCH_WIDTH, D_MODEL), BRANCH_WIDTH ** -0.5 * DEEPNORM_BETA),
        'w_o': nrm(ks[19], (DEPTH, D_MODEL, D_MODEL), D_MODEL ** -0.5 * DEEPNORM_BETA),
        'ln1_g': gain(ks[20], (DEPTH, D_MODEL)),
        'ln1_b': nrm(ks[21], (DEPTH, D_MODEL), 0.02),
        'w_router': nrm(ks[22], (DEPTH, D_MODEL, N_EXPERTS), D_MODEL ** -0.5),
        'b_router': nrm(ks[23], (DEPTH, N_EXPERTS), 0.01),
        'w_exp1': nrm(ks[24], (DEPTH, N_EXPERTS, D_MODEL, 2 * D_FF), D_MODEL ** -0.5),
        'b_exp1': nrm(ks[25], (DEPTH, N_EXPERTS, 2 * D_FF), 0.02),
        'w_exp2': nrm(ks[26], (DEPTH, N_EXPERTS, D_FF, D_MODEL), D_FF ** -0.5 * DEEPNORM_BETA),
        'b_exp2': nrm(ks[27], (DEPTH, N_EXPERTS, D_MODEL), 0.02),
        'ln2_g': gain(ks[28], (DEPTH, D_MODEL)),
        'ln2_b': nrm(ks[29], (DEPTH, D_MODEL), 0.02),
    }


def reference(x, positions, w_in, b_in, nsa_pe_k, nsa_pe_v, nsa_cmp_w1_k, nsa_cmp_w2_k,
              nsa_cmp_w1_v, nsa_cmp_w2_v, gmlp_ln_g, gmlp_ln_b, gmlp_w_s, gmlp_b_s,
              conv_w, conv_b, conv_ln_g, conv_ln_b, w_br, w_o, ln1_g, ln1_b,
              w_router, b_router, w_exp1, b_exp1, w_exp2, b_exp2, ln2_g, ln2_b):
    B, S, D = x.shape
    Dh = HEAD_DIM
    split_points = np.cumsum(SPLIT_SIZES)[:-1].tolist()

    def rope_kv(t):
        return partial_rope(t[:, :, None, :], positions)[:, :, 0, :]

    for l in range(DEPTH):
        h = x @ w_in[l] + b_in[l]
        (nq, nkc, nvc, nks, nvs, nkw, nvw, ngate,
         fq, fk, fv, ff, gu, gv, ca, cb, mg) = jnp.split(h, split_points, axis=-1)
        o_nsa = nsa_mixer(partial_rope(nq.reshape(B, S, NSA_HEADS, Dh), positions),
                          rope_kv(nkc), nvc, rope_kv(nks), nvs, rope_kv(nkw), nvw, ngate,
                          nsa_pe_k[l], nsa_pe_v[l], nsa_cmp_w1_k[l], nsa_cmp_w2_k[l],
                          nsa_cmp_w1_v[l], nsa_cmp_w2_v[l])
        o_fox = fox_mixer(fq.reshape(B, S, FOX_HEADS, Dh), fk.reshape(B, S, FOX_HEADS, Dh),
                          fv.reshape(B, S, FOX_HEADS, Dh), ff)
        o_gmlp = gmlp_mixer(gu, gv, gmlp_w_s[l], gmlp_b_s[l], gmlp_ln_g[l], gmlp_ln_b[l])
        o_conv = conv_mixer(ca, cb, conv_w[l], conv_b[l], conv_ln_g[l], conv_ln_b[l])
        branches = jnp.stack([o_nsa, o_fox, o_gmlp, o_conv], axis=2)
        proj = jnp.einsum('bsnc,ncd->bsnd', branches, w_br[l])
        gate = jax.nn.sigmoid(mg.reshape(B, S, N_BRANCH, D))
        mixed = jnp.sum(gate * proj, axis=2) @ w_o[l]
        x = layer_norm(DEEPNORM_ALPHA * x + mixed, ln1_g[l], ln1_b[l])
        ffn = moe_ffn(x, w_router[l], b_router[l], w_exp1[l], b_exp1[l], w_exp2[l], b_exp2[l])
        x = layer_norm(DEEPNORM_ALPHA * x + ffn, ln2_g[l], ln2_b[l])
    return x
```

```python
import contextlib
import numpy as np
import concourse.bass as bass
import concourse.mybir as mybir
from concourse.bass_utils import run_bass_kernel_spmd

F32 = mybir.dt.float32
BF16 = mybir.dt.bfloat16
I32 = mybir.dt.int32
U32 = mybir.dt.uint32
AF = mybir.ActivationFunctionType
ALU = mybir.AluOpType

S = 4096
D = 1024
NT = S // 128
NG = S // 512
L = 4
NIN = 6544
CAP = 640
NE = 32
ALPHA = float((2 * L) ** 0.25)
EPS = 1e-5
NEG = -30000.0
BIG = 32768.0
O_NQ, O_NKC, O_NVC, O_NKS, O_NVS, O_NKW, O_NVW, O_NG, O_FQ, O_FK, O_FV, O_FF, O_GU, O_GV, O_CA, O_CB, O_MG = (
    0, 256, 320, 384, 448, 512, 576, 640, 652, 908, 1164, 1420, 1424, 1680, 1936, 2192, 2448)
SEM_MAX = 30000
DMA_MAX = 1800
SAME_SYNC = True


class Buf:
    def __init__(self, h=None):
        self.h = h
        self.ws = {}
        self.rs = {}
        self.prs = {}

    def __getitem__(self, idx):
        return self.h[idx]


class Eng:
    def __init__(self, name, e):
        self.name = name
        self.e = e
        self.sem = None
        self.cnt = 0
        self.known = {}


def _merge(d, s):
    for kk, v in s.items():
        if d.get(kk, 0) < v:
            d[kk] = v


class KB:
    def __init__(self, nc):
        self.nc = nc
        self.es = contextlib.ExitStack()
        self.E = {n: Eng(n, e) for n, e in (("pe", nc.tensor), ("act", nc.scalar), ("dve", nc.vector),
                                            ("pool", nc.gpsimd), ("sp", nc.sync))}
        self.sems = []
        self.semown = []
        self.lanes = {"sp": [], "pool": [], "act": []}
        self.lane_i = {"sp": 0, "pool": 0, "act": 0}
        self.nl = 6
        self.ninst = 0

    def new_sem(self, owner):
        s = self.es.enter_context(self.nc.semaphore("s%d" % len(self.sems)))
        self.sems.append(s)
        self.semown.append(owner)
        return len(self.sems) - 1

    def _wait(self, E, deps):
        for si, val in deps.items():
            if self.semown[si] == E.name and (E.name == "pe" or not SAME_SYNC):
                continue
            if E.known.get(si, 0) < val:
                E.e.wait_ge(self.sems[si], val)
                E.known[si] = val
                self.ninst += 1

    def _deps(self, R, W, multi):
        deps = {}
        for b in R:
            _merge(deps, b.ws)
        for b in W:
            if b.rs:
                b.prs = b.rs
                b.rs = {}
                b.ws = {}
            _merge(deps, b.prs)
            if not multi:
                _merge(deps, b.ws)
        return deps

    def _commit(self, R, W, multi, si, val):
        for b in R:
            if b.rs.get(si, 0) < val:
                b.rs[si] = val
        for b in W:
            if multi:
                if b.ws.get(si, 0) < val:
                    b.ws[si] = val
            else:
                b.ws = {si: val}

    def op(self, en, thunk, R=(), W=(), multi=False):
        E = self.E[en]
        P = []
        for b in list(R) + list(W):
            if getattr(b, "psum", False) and b not in P:
                P.append(b)
        R = [b for b in R if not getattr(b, "psum", False)]
        W = [b for b in W if not getattr(b, "psum", False)]
        deps = self._deps(R, W, multi)
        _merge(deps, self._deps([], P, False))
        self._wait(E, deps)
        if E.sem is None or E.cnt >= SEM_MAX:
            E.sem = self.new_sem(en)
            E.cnt = 0
        ins = thunk()
        E.cnt += 1
        ins.then_inc(self.sems[E.sem], 1)
        self.ninst += 1
        self._commit(R, W, multi, E.sem, E.cnt)
        self._commit([], P, False, E.sem, E.cnt)

    def dma(self, q, out, in_, R=(), W=(), multi=True, indirect=None, **kw):
        E = self.E[q]
        lanes = self.lanes[q]
        if len(lanes) < self.nl:
            lanes.append([self.new_sem("dma"), 0])
            ln = lanes[-1]
        else:
            ln = lanes[self.lane_i[q] % self.nl]
            self.lane_i[q] += 1
        deps = self._deps(R, W, multi)
        if ln[1] > 0:
            _merge(deps, {ln[0]: 16 * ln[1]})
        self._wait(E, deps)
        if ln[1] >= DMA_MAX:
            ln[0] = self.new_sem("dma")
            ln[1] = 0
        if indirect is not None:
            ins = E.e.indirect_dma_start(out=out, in_=in_, **indirect)
        else:
            ins = E.e.dma_start(out=out, in_=in_, **kw)
        ln[1] += 1
        ins.then_inc(self.sems[ln[0]], 16)
        self.ninst += 1
        self._commit(R, W, multi, ln[0], 16 * ln[1])

    def barrier(self):
        ev = {}
        for E in self.E.values():
            if E.sem is not None and E.cnt > 0:
                ev[E.sem] = E.cnt
        for q in self.lanes:
            for ln in self.lanes[q]:
                if ln[1] > 0:
                    ev[ln[0]] = 16 * ln[1]
        for E in self.E.values():
            for si, val in ev.items():
                if self.semown[si] == E.name and E.name != "dma":
                    continue
                if E.known.get(si, 0) < val:
                    E.e.wait_ge(self.sems[si], val)
                    E.known[si] = val

    def mm(self, out, lhsT, rhs, start, stop, R, W, **kw):
        self.op("pe", lambda: self.nc.tensor.matmul(out, lhsT=lhsT, rhs=rhs, start=start, stop=stop, **kw), R, W)

    def tr(self, out, in_, ident, R, W):
        self.op("pe", lambda: self.nc.tensor.transpose(out, in_, ident), R, W)

    def act(self, out, in_, func, R, W, multi=False, **kw):
        self.op("act", lambda: self.nc.scalar.activation(out=out, in_=in_, func=func, **kw), R, W, multi)

    def tt(self, out, in0, in1, op, R, W, eng="dve", multi=False):
        e = self.nc.vector if eng == "dve" else self.nc.gpsimd
        self.op(eng, lambda: e.tensor_tensor(out=out, in0=in0, in1=in1, op=op), R, W, multi)

    def ts(self, out, in0, s1, s2, op0, op1, R, W, eng="dve", multi=False, **kw):
        e = self.nc.vector if eng == "dve" else self.nc.gpsimd
        if op1 is None:
            self.op(eng, lambda: e.tensor_scalar(out=out, in0=in0, scalar1=s1, scalar2=None, op0=op0, **kw), R, W, multi)
        else:
            self.op(eng, lambda: e.tensor_scalar(out=out, in0=in0, scalar1=s1, scalar2=s2, op0=op0, op1=op1, **kw), R, W, multi)

    def stt(self, out, in0, scalar, in1, op0, op1, R, W, multi=False):
        self.op("dve", lambda: self.nc.vector.scalar_tensor_tensor(out=out, in0=in0, scalar=scalar, in1=in1,
                                                                   op0=op0, op1=op1), R, W, multi)

    def cp(self, out, in_, R, W, eng="dve", multi=False):
        if eng == "act":
            self.act(out, in_, AF.Copy, R, W, multi)
        else:
            e = self.nc.vector if eng == "dve" else self.nc.gpsimd
            self.op(eng, lambda: e.tensor_copy(out=out, in_=in_), R, W, multi)

    def memset(self, ap, val, W, eng="dve", multi=False):
        e = self.nc.vector if eng == "dve" else self.nc.gpsimd
        self.op(eng, lambda: e.memset(ap, val), (), W, multi)


CONST_SPECS = {}


def host_consts():
    c = {}
    c["ident"] = np.eye(128, dtype=np.float32)
    kq = np.arange(128)
    c["m_causal"] = np.where(kq[:, None] <= kq[None, :], 0.0, NEG).astype(np.float32)
    c["m_band"] = np.where(kq[:, None] > kq[None, :], 0.0, NEG).astype(np.float32)
    c["m_tril"] = (kq[:, None] <= kq[None, :]).astype(np.float32)
    c["u_strict"] = (kq[:, None] < kq[None, :]).astype(np.float32)
    ncmp = 255
    cs = np.arange(256) * 16
    ss = np.arange(64) * 64
    ov = ((cs[:, None] <= ss[None, :] + 63) & (cs[:, None] + 31 >= ss[None, :])).astype(np.float32)
    ov[255] = 0
    c["ovl"] = np.concatenate([np.ones((256, 1), np.float32), ov], axis=1).reshape(2, 128, 65).transpose(1, 0, 2).copy()
    cend = np.arange(256) * 16 + 31
    cm = np.zeros((NT, 128, 2, 128), np.float32)
    for i in range(NT):
        tq = i * 128 + np.arange(128)
        for ct in range(2):
            ce = cend[ct * 128:(ct + 1) * 128]
            ok = (ce[:, None] <= tq[None, :]) & ((np.arange(128) + ct * 128)[:, None] < ncmp)
            cm[i, :, ct, :] = np.where(ok, 0.0, NEG)
    c["cmask"] = cm
    st = np.zeros((NT, 128, 2, 64), np.float32)
    j = np.arange(64)
    for i in range(NT):
        tq = i * 128 + np.arange(128)
        cur = (tq // 64)[:, None]
        causal = j[None, :] <= cur
        forced = (j[None, :] == 0) | (causal & (j[None, :] > cur - 2))
        st[i, :, 0, :] = causal
        st[i, :, 1, :] = (causal.astype(np.float32) - 1.0) + forced * 1e9
    c["seltab"] = st
    eb = np.zeros((64, S), np.float32)
    eb[np.arange(S) // 64, np.arange(S)] = BIG
    c["ebig"] = eb
    half = 8
    invf = (500000.0 ** (-np.arange(half, dtype=np.float32) / half)).astype(np.float32)
    fr = np.zeros((128, 1), np.float32)
    sg = np.ones((128, 1), np.float32)
    for r in range(128):
        rr = r % 64
        if rr < 16:
            fr[r, 0] = invf[rr % 8]
            if rr < 8:
                sg[r, 0] = -1.0
    c["ropef"] = np.concatenate([fr, sg], axis=1)
    c["iota32"] = np.tile(np.arange(32, dtype=np.float32)[None, :], (128, 1))
    return c


def build(nlayers=L, debug=False, stop=None):
    LW = nlayers
    lvl = None
    if stop is not None and ':' in stop:
        stop, lvl = stop.split(':')
        lvl = float(lvl)
    NEW = NE if stop in (None, 'E') else 1
    nc = bass.Bass("TRN2", target_bir_lowering=False)
    k = KB(nc)
    dbg = {}

    def din(name, shape, dt=F32):
        return Buf(nc.dram_tensor(name, list(shape), dt, kind="ExternalInput").ap())

    def dscr(name, shape, dt):
        kind = "ExternalOutput" if debug else "Internal"
        b = Buf(nc.dram_tensor(name, list(shape), dt, kind=kind).ap())
        dbg[name] = b
        return b

    cons = host_consts()
    xT_in = din("xT", [D, S])
    pos_in = din("pos", [1, S], I32)
    w_in = din("w_in", [LW, D, NIN])
    b_in = din("b_in", [LW, 1, NIN])
    w_sw = din("w_sw", [LW, D, 448])
    b_sw = din("b_sw", [LW, 1, 448])
    pe_kT = din("pe_kT", [LW, 64, 32])
    pe_vT = din("pe_vT", [LW, 64, 32])
    cw1k = din("cw1k", [LW, 2048, 64])
    cw2k = din("cw2k", [LW, 64, 64])
    cw1v = din("cw1v", [LW, 2048, 64])
    cw2v = din("cw2v", [LW, 64, 64])
    g_lng = din("g_lng", [LW, 1, 256])
    g_lnb = din("g_lnb", [LW, 1, 256])
    g_wsT = din("g_wsT", [LW, 128, 4, 128])
    g_bs = din("g_bs", [LW, 1, 512])
    c_wT = din("c_wT", [LW, 256, 31])
    c_par = din("c_par", [LW, 256, 3])
    w_br = din("w_br", [LW, 4, 256, D])
    w_o = din("w_o", [LW, D, D])
    ln1 = din("ln1", [LW, 128, 8, 2])
    ln2 = din("ln2", [LW, 128, 8, 2])
    w_r = din("w_r", [LW, D, NE])
    b_r = din("b_r", [LW, 1, NE])
    w1g = din("w1g", [LW, NEW, D, D])
    w1l = din("w1l", [LW, NEW, D, D])
    b1g = din("b1g", [LW, NEW, 1, D])
    b1l = din("b1l", [LW, NEW, 1, D])
    w2 = din("w2", [LW, NEW, D, D])
    b2 = din("b2", [LW, NEW, 1, D])
    cin = {n: din("c_" + n, list(v.shape)) for n, v in cons.items()}
    yT_out = Buf(nc.dram_tensor("yT", [D, S], F32, kind="ExternalOutput").ap())

    xres = dscr("xres", [D, S], F32)
    cosd = dscr("cosd", [128, S], F32)
    sind = dscr("sind", [128, S], F32)
    qn = dscr("qn", [4, 64, S], BF16)
    kcT = dscr("kcT", [64, S], BF16)
    vcT = dscr("vcT", [64, S], BF16)
    ksT = dscr("ksT", [64, S], BF16)
    kwT = dscr("kwT", [64, S], BF16)
    vaug = dscr("vaug", [S, 6, 65], BF16)
    gvn = dscr("gvn", [S, 256], BF16)
    fqa = dscr("fqa", [4, 70, S], BF16)
    fka = dscr("fka", [4, 70, S], BF16)
    guT = dscr("guT", [256, S], BF16)
    hcT = dscr("hcT", [256, 32 + S], F32)
    oT = dscr("oT", [4, 256, S], BF16)
    mixT = dscr("mixT", [D, S], BF16)
    xe_d = dscr("xe_d", [NE * CAP, D], BF16)
    yr_d = dscr("yr_d", [NE * CAP, D], BF16)

    es = k.es
    with es:
        def sb(name, shape, dt=F32):
            return Buf(es.enter_context(nc.sbuf_tensor(name, list(shape), dt)))

        psum = es.enter_context(nc.psum_tensor("psum", [128, 8, 512], F32))
        PB = [Buf(psum) for _ in range(8)]
        for b_ in PB:
            b_.psum = True

        def pb(i, n=512, p0=0, p1=128):
            return psum[p0:p1, i, 0:n]

        def pbb(i):
            return psum[:, i, :].bitcast(BF16)

        ident_f = sb("ident_f", [128, 128])
        ident_b = sb("ident_b", [128, 128], BF16)
        m_causal = sb("m_causal", [128, 128])
        m_band = sb("m_band", [128, 128])
        ones_b = sb("ones_b", [128, 512], BF16)
        zeros_b = sb("zeros_b", [1, 512], BF16)
        ones_f = sb("ones_f", [128, 512])
        onesD = sb("onesD", [128, 128])
        onesC = sb("onesC", [128, 128])
        ropef = sb("ropef", [128, 2])
        xT_bf = sb("xT_bf", [128, 8, S], BF16)
        gates_r = sb("gates_r", [128, NT, 12])
        k.dma("sp", ident_f[:, :], cin["ident"][:, :], [cin["ident"]], [ident_f])
        k.dma("sp", m_causal[:, :], cin["m_causal"][:, :], [], [m_causal])
        k.dma("sp", m_band[:, :], cin["m_band"][:, :], [], [m_band])
        k.dma("sp", ropef[:, :], cin["ropef"][:, :], [], [ropef])
        k.cp(ident_b[:, :], ident_f[:, :], [ident_f], [ident_b])
        k.memset(ones_b[:, :], 1.0, [ones_b])
        k.memset(zeros_b[:, :], 0.0, [zeros_b])
        k.memset(ones_f[:, :], 1.0, [ones_f])
        k.memset(onesD[:, :], 1.0 / D, [onesD])
        k.memset(onesC[:, :], 1.0 / 256, [onesC])
        for c in range(8):
            k.dma("sp", xres[c * 128:(c + 1) * 128, :], xT_in[c * 128:(c + 1) * 128, :], [xT_in], [xres])
            k.dma("pool", xT_bf[:, c, :], xT_in[c * 128:(c + 1) * 128, :], [xT_in], [xT_bf], max_dma_last_dim=4096)
        with contextlib.ExitStack() as ps_:
            zt = Buf(ps_.enter_context(nc.sbuf_tensor("zt", [128, 32], F32)))
            k.memset(zt[:, :], 0.0, [zt])
            for ct in range(2):
                k.dma("sp", hcT[ct * 128:(ct + 1) * 128, 0:32], zt[:, :], [zt], [hcT])
            pi_ = Buf(ps_.enter_context(nc.sbuf_tensor("rpi", [128, 512], I32)))
            a = Buf(ps_.enter_context(nc.sbuf_tensor("rpa", [128, 512], F32)))
            b_ = Buf(ps_.enter_context(nc.sbuf_tensor("rpb", [128, 512], F32)))
            ki = Buf(ps_.enter_context(nc.sbuf_tensor("rpk", [128, 512], I32)))
            t1 = Buf(ps_.enter_context(nc.sbuf_tensor("rpt", [128, 512], F32)))
            for g in range(NG):
                k.dma("sp", pi_[:, :], pos_in[0:1, g * 512:(g + 1) * 512].partition_broadcast(128), [pos_in], [pi_])
                k.cp(a[:, :], pi_[:, :], [pi_], [a])
                k.ts(a[:, :], a[:, :], ropef[:, 0:1], None, ALU.mult, None, [a, ropef], [a])
                for which, dst in ((0, sind), (1, cosd)):
                    k.ts(b_[:, :], a[:, :], float(np.pi / 2) if which else 0.0, 1.0 / (2 * np.pi), ALU.add, ALU.mult, [a], [b_])
                    k.cp(ki[:, :], b_[:, :], [b_], [ki])
                    k.cp(b_[:, :], ki[:, :], [ki], [b_])
                    k.stt(b_[:, :], b_[:, :], float(-2 * np.pi), a[:, :], ALU.mult, ALU.add, [a, b_], [b_])
                    if which:
                        k.ts(b_[:, :], b_[:, :], float(np.pi / 2), None, ALU.add, None, [b_], [b_])
                    k.ts(t1[:, :], b_[:, :], float(np.pi), float(-2 * np.pi), ALU.is_gt, ALU.mult, [b_], [t1])
                    k.tt(b_[:, :], b_[:, :], t1[:, :], ALU.add, [b_, t1], [b_])
                    k.ts(t1[:, :], b_[:, :], float(-np.pi), float(2 * np.pi), ALU.is_lt, ALU.mult, [b_], [t1])
                    k.tt(b_[:, :], b_[:, :], t1[:, :], ALU.add, [b_, t1], [b_])
                    k.act(t1[:, :], b_[:, :], AF.Sin, [b_], [t1])
                    if which == 0:
                        k.ts(t1[:, :], t1[:, :], ropef[:, 1:2], None, ALU.mult, None, [t1, ropef], [t1])
                    k.dma("sp", dst[:, g * 512:(g + 1) * 512], t1[:, :], [t1], [dst])
            k.barrier()

        def gelu(out, z, t1, t2, R, W, Tt1, Tt2):
            k.act(t1, z, AF.Square, R, [Tt1])
            k.ts(t1, t1, 0.044715, 1.0, ALU.mult, ALU.add, [Tt1], [Tt1])
            k.tt(t1, t1, z, ALU.mult, [Tt1] + R, [Tt1])
            k.act(t2, t1, AF.Sigmoid, [Tt1], [Tt2], scale=1.5957691216057308)
            k.tt(out, t2, z, ALU.mult, [Tt2] + R, W)

        def ln_featmajor(xs, n, onesM, scr, bank_m, bank_e, out_fn):
            sq, mean, rstd = scr["sq"], scr["mean"], scr["rstd"]
            nchunk = len(xs)
            for i, (xa, xb) in enumerate(xs):
                k.mm(pb(bank_m, n), onesM[:, :], xa, i == 0, i == nchunk - 1, [onesM, xb], [PB[bank_m]])
            for i, (xa, xb) in enumerate(xs):
                k.act(sq[:, 0:n], xa, AF.Square, [xb], [sq])
                k.mm(pb(bank_e, n), onesM[:, :], sq[:, 0:n], i == 0, i == nchunk - 1, [onesM, sq], [PB[bank_e]])
            k.cp(mean[:, 0:n], pb(bank_m, n), [PB[bank_m]], [mean], eng="act")
            k.tt(rstd[:, 0:n], mean[:, 0:n], mean[:, 0:n], ALU.mult, [mean], [rstd])
            k.tt(rstd[:, 0:n], pb(bank_e, n), rstd[:, 0:n], ALU.subtract, [PB[bank_e], rstd], [rstd])
            k.act(rstd[:, 0:n], rstd[:, 0:n], AF.Sqrt, [rstd], [rstd], bias=EPS)
            k.op("dve", lambda: nc.vector.reciprocal(rstd[:, 0:n], rstd[:, 0:n]), [rstd], [rstd])
            for i, (xa, xb) in enumerate(xs):
                out_fn(i, xa, xb, mean[:, 0:n], rstd[:, 0:n], mean, rstd)

        for l in range(nlayers):
            last = (l == nlayers - 1)
            with contextlib.ExitStack() as pa:
                def psb(name, shape, dt=F32):
                    return Buf(pa.enter_context(nc.sbuf_tensor(name + "_%d" % l, list(shape), dt)))
                NW = 3
                wt = [psb("wt%d" % i, [128, 8, 128], BF16) for i in range(NW)]
                wt2 = [psb("wt2_%d" % i, [128, 8, 128], BF16) for i in range(NW)]
                br = [psb("br%d" % i, [1, 256], BF16) for i in range(NW)]
                cosb = [psb("cosb%d" % i, [128, 512]) for i in range(2)]
                sinb = [psb("sinb%d" % i, [128, 512]) for i in range(2)]
                stg = [psb("stg%d" % i, [128, 512], BF16) for i in range(3)]
                stgf = [psb("stgf%d" % i, [128, 512]) for i in range(3)]
                t1s = [psb("t1s%d" % i, [128, 512]) for i in range(2)]
                t2s = [psb("t2s%d" % i, [128, 512]) for i in range(2)]
                wi = w_in.h[l].rearrange("(c p) n -> p c n", p=128)
                wsw = w_sw.h[l].rearrange("(c p) n -> p c n", p=128)
                cnt = {"w": 0, "pb": 0, "s": 0, "t": 0}

                def load_w(dst, dstb, boff, srcw, srcb, srcB, srcbB, c0, n, roff=0):
                    k.dma("pool", dst[:, :, roff:roff + n], srcw[:, :, c0:c0 + n], [srcB], [dst])
                    k.dma("pool", dstb[0:1, boff + roff:boff + roff + n], srcb[0:1, c0:c0 + n], [srcbB], [dstb])

                def proj(bank, w, bt, boff, nrows, g):
                    for c in range(8):
                        k.mm(pb(bank, 512, 0, nrows), w[:, c, 0:nrows], xT_bf[:, c, g * 512:(g + 1) * 512], c == 0, False,
                             [w, xT_bf], [PB[bank]])
                    k.mm(pb(bank, 512, 0, nrows), bt[0:1, boff:boff + nrows], ones_b[0:1, 0:512], False, True,
                         [bt, ones_b], [PB[bank]])

                def rope_tile(c0, s0, nrows, writer):
                    i = cnt["w"] % NW
                    cnt["w"] += 1
                    load_w(wt[i], br[i], 0, wi, b_in.h[l], w_in, b_in, c0, nrows)
                    load_w(wt2[i], br[i], 128, wsw, b_sw.h[l], w_sw, b_sw, s0, nrows)
                    for g in range(NG):
                        ci = g % 2
                        k.dma("sp", cosb[ci][0:nrows, :], cosd[0:nrows, g * 512:(g + 1) * 512], [cosd], [cosb[ci]])
                        k.dma("sp", sinb[ci][0:nrows, :], sind[0:nrows, g * 512:(g + 1) * 512], [sind], [sinb[ci]])
                        b0 = cnt["pb"] % 2 * 2
                        cnt["pb"] += 1
                        proj(b0, wt[i], br[i], 0, nrows, g)
                        proj(b0 + 1, wt2[i], br[i], 128, nrows, g)
                        ti = cnt["t"] % 2
                        cnt["t"] += 1
                        si = cnt["s"] % 3
                        cnt["s"] += 1
                        k.tt(t1s[ti][0:nrows, :], pb(b0, 512, 0, nrows), cosb[ci][0:nrows, :], ALU.mult, [PB[b0], cosb[ci]], [t1s[ti]])
                        k.tt(t2s[ti][0:nrows, :], pb(b0 + 1, 512, 0, nrows), sinb[ci][0:nrows, :], ALU.mult, [PB[b0 + 1], sinb[ci]], [t2s[ti]])
                        k.tt(stg[si][0:nrows, :], t1s[ti][0:nrows, :], t2s[ti][0:nrows, :], ALU.add, [t1s[ti], t2s[ti]], [stg[si]])
                        writer(stg[si], g)

                def wr_q(hh):
                    def f(st_, g):
                        k.dma("sp", qn[2 * hh, :, g * 512:(g + 1) * 512], st_[0:64, :], [st_], [qn])
                        k.dma("sp", qn[2 * hh + 1, :, g * 512:(g + 1) * 512], st_[64:128, :], [st_], [qn])
                    return f
                rope_tile(O_NQ, 0, 128, wr_q(0))
                rope_tile(O_NQ + 128, 128, 128, wr_q(1))

                def wr_k(d0, d1):
                    def f(st_, g):
                        k.dma("sp", d0[:, g * 512:(g + 1) * 512], st_[0:64, :], [st_], [d0])
                        if d1 is not None:
                            k.dma("sp", d1[:, g * 512:(g + 1) * 512], st_[64:128, :], [st_], [d1])
                    return f
                rope_tile(O_NKC, 256, 64, wr_k(kcT, None))
                rope_tile(O_NKS, 320, 64, wr_k(ksT, None))
                rope_tile(O_NKW, 384, 64, wr_k(kwT, None))

                def plain_tile(c0, nrows, post):
                    i = cnt["w"] % NW
                    cnt["w"] += 1
                    load_w(wt[i], br[i], 0, wi, b_in.h[l], w_in, b_in, c0, nrows)
                    for g in range(NG):
                        b0 = cnt["pb"] % 4
                        cnt["pb"] += 1
                        proj(b0, wt[i], br[i], 0, nrows, g)
                        post(b0, g)

                def post_copy(dsts, scale):
                    def f(b0, g):
                        si = cnt["s"] % 3
                        cnt["s"] += 1
                        nr = 64 * len(dsts)
                        k.act(stg[si][0:nr, :], pb(b0, 512, 0, nr), AF.Copy, [PB[b0]], [stg[si]], scale=scale)
                        for j, (dd, sl) in enumerate(dsts):
                            k.dma("sp", sl(g), stg[si][64 * j:64 * j + 64, :], [stg[si]], [dd])
                    return f
                plain_tile(O_NVC, 64, post_copy([(vcT, lambda g: vcT[:, g * 512:(g + 1) * 512])], 1.0))
                for hh in range(2):
                    plain_tile(O_FQ + 128 * hh, 128, post_copy(
                        [(fqa, (lambda h_: (lambda g: fqa[h_, 0:64, g * 512:(g + 1) * 512]))(2 * hh)),
                         (fqa, (lambda h_: (lambda g: fqa[h_, 0:64, g * 512:(g + 1) * 512]))(2 * hh + 1))], 0.125))
                    plain_tile(O_FK + 128 * hh, 128, post_copy(
                        [(fka, (lambda h_: (lambda g: fka[h_, 0:64, g * 512:(g + 1) * 512]))(2 * hh)),
                         (fka, (lambda h_: (lambda g: fka[h_, 0:64, g * 512:(g + 1) * 512]))(2 * hh + 1))], 1.0))

                def post_gelu(ct):
                    def f(b0, g):
                        si = cnt["s"] % 3
                        cnt["s"] += 1
                        ti = cnt["t"] % 2
                        cnt["t"] += 1
                        gelu(stg[si][:, :], pb(b0), t1s[ti][:, :], t2s[ti][:, :], [PB[b0]], [stg[si]], t1s[ti], t2s[ti])
                        k.dma("sp", guT[ct * 128:(ct + 1) * 128, g * 512:(g + 1) * 512], stg[si][:, :], [stg[si]], [guT])
                    return f
                for ct in range(2):
                    plain_tile(O_GU + 128 * ct, 128, post_gelu(ct))

                for ct in range(2):
                    i = cnt["w"] % NW
                    cnt["w"] += 1
                    load_w(wt[i], br[i], 0, wi, b_in.h[l], w_in, b_in, O_CA + 128 * ct, 128)
                    load_w(wt2[i], br[i], 128, wi, b_in.h[l], w_in, b_in, O_CB + 128 * ct, 128)
                    for g in range(NG):
                        b0 = cnt["pb"] % 2 * 2
                        cnt["pb"] += 1
                        proj(b0, wt[i], br[i], 0, 128, g)
                        proj(b0 + 1, wt2[i], br[i], 128, 128, g)
                        ti = cnt["t"] % 2
                        cnt["t"] += 1
                        si = cnt["s"] % 3
                        cnt["s"] += 1
                        k.act(t1s[ti][:, :], pb(b0 + 1), AF.Sigmoid, [PB[b0 + 1]], [t1s[ti]])
                        k.tt(stgf[si][:, :], pb(b0), t1s[ti][:, :], ALU.mult, [PB[b0], t1s[ti]], [stgf[si]])
                        k.dma("sp", hcT[ct * 128:(ct + 1) * 128, 32 + g * 512:32 + (g + 1) * 512], stgf[si][:, :], [stgf[si]], [hcT])

                i = cnt["w"] % NW
                cnt["w"] += 1
                load_w(wt[i], br[i], 0, wi, b_in.h[l], w_in, b_in, O_FF, 4)
                cum = [psb("cum%d" % j, [4, 512]) for j in range(2)]
                ffe = psb("ffe", [4, 512])
                aug = [psb("aug%d" % j, [4, 12, 512], BF16) for j in range(2)]
                r1 = psb("ffr1", [4, 512])
                r2 = psb("ffr2", [4, 512])
                for j in range(2):
                    k.memset(aug[j][:, 3:9, :], 1.0, [aug[j]])
                for g in range(NG):
                    b0 = cnt["pb"] % 4
                    cnt["pb"] += 1
                    proj(b0, wt[i], br[i], 0, 4, g)
                    k.act(ffe[:, :], pb(b0, 512, 0, 4), AF.Exp, [PB[b0]], [ffe], scale=-1.0)
                    k.act(ffe[:, :], ffe[:, :], AF.Ln, [ffe], [ffe], bias=1.0)
                    cj = cum[g % 2]
                    cp_ = cum[(g + 1) % 2]
                    init = 0.0 if g == 0 else cp_[:, 511:512]
                    k.op("dve", lambda: nc.vector.tensor_tensor_scan(out=cj[:, :], data0=ones_f[0:4, 0:512], data1=ffe[:, :],
                                                                      initial=init, op0=ALU.mult, op1=ALU.subtract),
                         [ones_f, ffe, cp_], [cj])
                    ag = aug[g % 2]
                    k.cp(ag[:, 0, :], cj[:, :], [cj], [ag], multi=True)
                    k.tt(r1[:, :], cj[:, :], ag[:, 0, :], ALU.subtract, [cj, ag], [r1])
                    k.cp(ag[:, 1, :], r1[:, :], [r1], [ag], multi=True)
                    k.tt(r2[:, :], r1[:, :], ag[:, 1, :], ALU.subtract, [r1, ag], [r2])
                    k.cp(ag[:, 2, :], r2[:, :], [r2], [ag], multi=True)
                    k.ts(ag[:, 9:12, :], ag[:, 0:3, :], -1.0, None, ALU.mult, None, [ag], [ag], multi=True)
                    for h_ in range(4):
                        k.dma("sp", fqa[h_, 64:70, g * 512:(g + 1) * 512], ag[h_:h_ + 1, 0:6, :], [ag], [fqa])
                        k.dma("sp", fka[h_, 64:70, g * 512:(g + 1) * 512], ag[h_:h_ + 1, 6:12, :], [ag], [fka])

                wtm = psb("wtm", [128, 8, 652], BF16)
                btm = psb("btm", [1, 652], BF16)
                offs = 0
                for (c0, n) in ((O_NVS, 64), (O_NVW, 64), (O_FV, 256), (O_NG, 12), (O_GV, 256)):
                    k.dma("pool", wtm[:, :, offs:offs + n], wi[:, :, c0:c0 + n], [w_in], [wtm])
                    k.dma("pool", btm[0:1, offs:offs + n], b_in.h[l][0:1, c0:c0 + n], [b_in], [btm])
                    offs += n
                lng = psb("lng", [128, 256])
                lnb = psb("lnb", [128, 256])
                k.dma("sp", lng[:, :], g_lng.h[l][0:1, :].partition_broadcast(128), [g_lng], [lng])
                k.dma("sp", lnb[:, :], g_lnb.h[l][0:1, :].partition_broadcast(128), [g_lnb], [lnb])
                vst = [psb("vst%d" % j, [128, 6, 65], BF16) for j in range(2)]
                gvb = [psb("gvb%d" % j, [128, 256], BF16) for j in range(2)]
                gv1 = psb("gv1", [128, 256])
                gv2 = psb("gv2", [128, 256])
                gv3 = psb("gv3", [128, 256])
                bst = psb("bst", [128, 6])
                mv = psb("mv", [128, 2])
                for j in range(2):
                    k.memset(vst[j][:, :, 64:65], 1.0, [vst[j]])
                for tt_ in range(NT):
                    bA = 4 + (tt_ % 2) * 2
                    bB = bA + 1
                    for (bk, o0, n) in ((bA, 0, 384), (bB, 384, 268)):
                        for c in range(8):
                            k.mm(pb(bk, n), xT_bf[:, c, tt_ * 128:(tt_ + 1) * 128], wtm[:, c, o0:o0 + n], c == 0, False,
                                 [xT_bf, wtm], [PB[bk]])
                        k.mm(pb(bk, n), ones_b[0:1, 0:128], btm[0:1, o0:o0 + n], False, True, [ones_b, btm], [PB[bk]])
                    vs_ = vst[tt_ % 2]
                    k.cp(vs_[:, :, 0:64], psum[:, bA, 0:384].rearrange("p (a b) -> p a b", b=64), [PB[bA]], [vs_], eng="act", multi=True)
                    k.dma("sp", vaug[tt_ * 128:(tt_ + 1) * 128, :, :], vs_[:, :, :], [vs_], [vaug])
                    k.act(gates_r[:, tt_, :], pb(bB, 12), AF.Sigmoid, [PB[bB]], [gates_r], multi=True)
                    z = psum[:, bB, 12:268]
                    gelu(gv3[:, :], z, gv1[:, :], gv2[:, :], [PB[bB]], [gv3], gv1, gv2)
                    k.op("dve", lambda: nc.vector.bn_stats(out=bst[:, :], in_=gv3[:, :]), [gv3], [bst])
                    k.op("dve", lambda: nc.vector.bn_aggr(out=mv[:, :], in_=bst[:, :]), [bst], [mv])
                    k.act(mv[:, 1:2], mv[:, 1:2], AF.Sqrt, [mv], [mv], bias=EPS)
                    k.op("dve", lambda: nc.vector.reciprocal(mv[:, 1:2], mv[:, 1:2]), [mv], [mv])
                    k.ts(gv1[:, :], gv3[:, :], mv[:, 0:1], mv[:, 1:2], ALU.subtract, ALU.mult, [gv3, mv], [gv1])
                    k.tt(gv1[:, :], gv1[:, :], lng[:, :], ALU.mult, [gv1, lng], [gv1])
                    gb = gvb[tt_ % 2]
                    k.tt(gb[:, :], gv1[:, :], lnb[:, :], ALU.add, [gv1, lnb], [gb])
                    k.dma("sp", gvn[tt_ * 128:(tt_ + 1) * 128, :], gb[:, :], [gb], [gvn])
            k.barrier()
            if stop == "A":
                break

            with contextlib.ExitStack() as pbk:
                def psb(name, shape, dt=F32):
                    return Buf(pbk.enter_context(nc.sbuf_tensor(name + "_%d" % l, list(shape), dt)))
                kcmpT = psb("kcmpT", [64, 256], BF16)
                vcaug = psb("vcaug", [128, 2, 129], BF16)
                k.dma("pool", vcaug[:, :, 64:129], cin["ovl"][:, :, :], [cin["ovl"]], [vcaug])
                with contextlib.ExitStack() as pc:
                    def csb(name, shape, dt=F32):
                        return Buf(pc.enter_context(nc.sbuf_tensor(name + "_%d" % l, list(shape), dt)))
                    kin = csb("kin", [64, S + 16], BF16)
                    k.memset(kin[:, S:S + 16], 0.0, [kin], multi=True)
                    w1b = csb("w1b", [64, 32, 64], BF16)
                    w2b = csb("w2b", [64, 64], BF16)
                    peT = csb("peT", [64, 32], BF16)
                    peB = csb("peB", [64, 32, 256], BF16)
                    g1 = csb("g1", [64, 256], BF16)
                    ct1 = csb("ct1", [64, 256])
                    ct2 = csb("ct2", [64, 256])
                    for which in range(2):
                        src, w1s, w2s, pes = ((kcT, cw1k, cw2k, pe_kT), (vcT, cw1v, cw2v, pe_vT))[which]
                        k.dma("sp", kin[:, 0:S], src[:, :], [src], [kin])
                        k.dma("pool", w1b[:, :, :], w1s.h[l].rearrange("(j d) h -> d j h", d=64), [w1s], [w1b])
                        k.dma("pool", w2b[:, :], w2s.h[l], [w2s], [w2b])
                        k.dma("pool", peT[:, :], pes.h[l], [pes], [peT])
                        k.cp(peB[:, :, :], peT[:, :].unsqueeze(2).to_broadcast([64, 32, 256]), [peT], [peB])
                        for j in range(32):
                            k.mm(pb(0, 256, 0, 64), w1b[:, j, :], kin[:, j:j + 16 * 255 + 1:16], j == 0, False, [w1b, kin], [PB[0]])
                        for j in range(32):
                            k.mm(pb(0, 256, 0, 64), w1b[:, j, :], peB[:, j, :], False, j == 31, [w1b, peB], [PB[0]])
                        gelu(g1[:, :], pb(0, 256, 0, 64), ct1[:, :], ct2[:, :], [PB[0]], [g1], ct1, ct2)
                        if which == 0:
                            k.mm(pb(1, 256, 0, 64), w2b[:, :], g1[:, :], True, True, [w2b, g1], [PB[1]])
                            k.cp(kcmpT[:, :], pb(1, 256, 0, 64), [PB[1]], [kcmpT])
                        else:
                            for ct in range(2):
                                k.mm(pb(2 + ct, 64), g1[:, ct * 128:(ct + 1) * 128], w2b[:, :], True, True, [g1, w2b], [PB[2 + ct]])
                                k.cp(vcaug[:, ct, 0:64], pb(2 + ct, 64), [PB[2 + ct]], [vcaug], multi=True)
                    k.barrier()
                if stop == "B1":
                    break

                qh = psb("qh", [64, 4, S], BF16)
                ksb = psb("ksb", [64, S], BF16)
                kwb = psb("kwb", [64, S], BF16)
                vsw = psb("vsw", [128, NT, 2, 65], BF16)
                ebig = psb("ebig", [64, S], BF16)
                k.dma("sp", qh[:, :, :], qn.h.rearrange("h d s -> d h s"), [qn], [qh])
                k.dma("sp", ksb[:, :], ksT[:, :], [ksT], [ksb])
                k.dma("sp", kwb[:, :], kwT[:, :], [kwT], [kwb])
                for t_ in range(NT):
                    k.dma("sp", vsw[:, t_, :, :], vaug[t_ * 128:(t_ + 1) * 128, 0:2, :], [vaug], [vsw])
                k.dma("pool", ebig[:, :], cin["ebig"][:, :], [cin["ebig"]], [ebig], max_dma_last_dim=4096)
                cmk = [psb("cmk%d" % j, [128, 2, 128]) for j in range(2)]
                stb = [psb("stb%d" % j, [128, 2, 64]) for j in range(2)]
                pT = [psb("pT%d" % j, [128, 4, 128], BF16) for j in range(3)]
                imp = psb("imp", [128, 64])
                imp2 = psb("imp2", [128, 64])
                mx = psb("mx", [128, 8])
                selm = psb("selm", [128, 64])
                selT = psb("selT", [64, 128], BF16)
                den = psb("den", [128, 12])
                coef = psb("coef", [128, 12])
                acc_c = psb("acc_c", [128, 4, 64])
                onsa = psb("onsa", [128, 256])
                onT = [psb("onT%d" % j, [128, 2, 128], BF16) for j in range(2)]
                npt = 0
                for i in range(NT if lvl is None else (0 if lvl < 2 else 1)):
                    q0 = i * 128
                    cm_ = cmk[i % 2]
                    st_ = stb[i % 2]
                    k.dma("sp", cm_[:, :, :], cin["cmask"][i, :, :, :], [cin["cmask"]], [cm_])
                    k.dma("sp", st_[:, :, :], cin["seltab"][i, :, :, :], [cin["seltab"]], [st_])
                    nct = 2 if i >= 16 else 1
                    for ct in range(nct):
                        sbk = 2 + (npt % 2)
                        k.mm(pb(sbk), kcmpT[:, ct * 128:(ct + 1) * 128], qh[:, :, q0:q0 + 128], True, True, [kcmpT, qh], [PB[sbk]])
                        k.tt(psum[:, sbk, :].rearrange("p (h q) -> p h q", h=4), psum[:, sbk, :].rearrange("p (h q) -> p h q", h=4),
                             cm_[:, ct, :].unsqueeze(1).to_broadcast([128, 4, 128]), ALU.add, [PB[sbk], cm_], [PB[sbk]])
                        p_ = pT[npt % 3]
                        npt += 1
                        k.act(p_[:, :, :], psum[:, sbk, :].rearrange("p (h q) -> p h q", h=4), AF.Exp, [PB[sbk]], [p_], scale=0.125)
                        if lvl is not None and lvl < 2.2:
                            continue
                        for h_ in range(4):
                            bk = h_ // 2
                            k.mm(psum[:, bk, (h_ % 2) * 129:(h_ % 2) * 129 + 129], p_[:, h_, :], vcaug[:, ct, :],
                                 (ct == 0 and h_ % 2 == 0), False, [p_, vcaug], [PB[bk]], skip_group_check=True)
                    if lvl is not None and lvl < 2.3:
                        continue
                    for h_ in range(4):
                        bk = h_ // 2
                        o_ = (h_ % 2) * 129
                        k.ts(den[:, h_:h_ + 1], psum[:, bk, o_ + 64:o_ + 65], 1e-30, None, ALU.max, None, [PB[bk]], [den], multi=True)
                    k.op("dve", lambda: nc.vector.reciprocal(coef[:, 0:4], den[:, 0:4]), [den], [coef], multi=True)
                    if lvl is not None and lvl < 2.4:
                        continue
                    for h_ in range(4):
                        bk = h_ // 2
                        o_ = (h_ % 2) * 129
                        k.cp(acc_c[:, h_, :], psum[:, bk, o_:o_ + 64], [PB[bk]], [acc_c], eng="act", multi=True)
                        if h_ == 0:
                            k.ts(imp[:, :], psum[:, bk, o_ + 65:o_ + 129], coef[:, 0:1], None, ALU.mult, None, [PB[bk], coef], [imp])
                        else:
                            k.stt(imp[:, :], psum[:, bk, o_ + 65:o_ + 129], coef[:, h_:h_ + 1], imp[:, :], ALU.mult, ALU.add,
                                  [PB[bk], coef, imp], [imp])
                    if lvl is not None and lvl < 3:
                        continue
                    k.tt(imp[:, :], imp[:, :], st_[:, 0, :], ALU.mult, [imp, st_], [imp])
                    k.tt(imp[:, :], imp[:, :], st_[:, 1, :], ALU.add, [imp, st_], [imp])
                    k.op("dve", lambda: nc.vector.max(out=mx[:, :], in_=imp[:, :]), [imp], [mx])
                    k.op("dve", lambda: nc.vector.match_replace(out=imp2[:, :], in_to_replace=mx[:, :], in_values=imp[:, :],
                                                                 imm_value=-2.0), [mx, imp], [imp2])
                    k.op("dve", lambda: nc.vector.max(out=mx[:, :], in_=imp2[:, :]), [imp2], [mx])
                    k.ts(selm[:, :], imp[:, :], mx[:, 7:8], None, ALU.is_ge, None, [imp, mx], [selm])
                    k.tt(selm[:, :], selm[:, :], st_[:, 0, :], ALU.mult, [selm, st_], [selm])
                    k.ts(selm[:, :], selm[:, :], -1.0, None, ALU.add, None, [selm], [selm])
                    k.tr(pb(7, 128, 0, 64), selm[:, :], ident_f[:, :], [selm, ident_f], [PB[7]])
                    k.cp(selT[:, :], pb(7, 128, 0, 64), [PB[7]], [selT])
                    if lvl is not None and lvl < 4:
                        continue
                    k.mm(pb(4, 260), zeros_b[0:1, 0:128], zeros_b[0:1, 0:260], True, False, [zeros_b], [PB[4]], skip_group_check=True)
                    for kt in range(i + 1):
                        sbk = 2 + (npt % 2)
                        k.mm(pb(sbk), ksb[:, kt * 128:(kt + 1) * 128], qh[:, :, q0:q0 + 128], True, False, [ksb, qh], [PB[sbk]])
                        k.mm(pb(sbk), ebig[:, kt * 128:(kt + 1) * 128], selT[:, :].unsqueeze(1).to_broadcast([64, 4, 128]), False, True,
                             [ebig, selT], [PB[sbk]])
                        if kt == i:
                            v4 = psum[:, sbk, :].rearrange("p (h q) -> p h q", h=4)
                            k.tt(v4, v4, m_causal[:, :].unsqueeze(1).to_broadcast([128, 4, 128]), ALU.add, [PB[sbk], m_causal], [PB[sbk]])
                        p_ = pT[npt % 3]
                        npt += 1
                        k.act(p_[:, :, :], psum[:, sbk, :].rearrange("p (h q) -> p h q", h=4), AF.Exp, [PB[sbk]], [p_], scale=0.125)
                        for h_ in range(4):
                            k.mm(psum[:, 4, h_ * 65:h_ * 65 + 65], p_[:, h_, :], vsw[:, kt, 0, :], False, False, [p_, vsw], [PB[4]],
                                 skip_group_check=True)
                    if lvl is not None and lvl < 5:
                        continue
                    k.mm(pb(5, 260), zeros_b[0:1, 0:128], zeros_b[0:1, 0:260], True, False, [zeros_b], [PB[5]], skip_group_check=True)
                    for kt in range(max(0, i - 4), i + 1):
                        sbk = 2 + (npt % 2)
                        k.mm(pb(sbk), kwb[:, kt * 128:(kt + 1) * 128], qh[:, :, q0:q0 + 128], True, True, [kwb, qh], [PB[sbk]])
                        if kt == i or kt == i - 4:
                            mk = m_causal if kt == i else m_band
                            v4 = psum[:, sbk, :].rearrange("p (h q) -> p h q", h=4)
                            k.tt(v4, v4, mk[:, :].unsqueeze(1).to_broadcast([128, 4, 128]), ALU.add, [PB[sbk], mk], [PB[sbk]])
                        p_ = pT[npt % 3]
                        npt += 1
                        k.act(p_[:, :, :], psum[:, sbk, :].rearrange("p (h q) -> p h q", h=4), AF.Exp, [PB[sbk]], [p_], scale=0.125)
                        for h_ in range(4):
                            k.mm(psum[:, 5, h_ * 65:h_ * 65 + 65], p_[:, h_, :], vsw[:, kt, 1, :], False, False, [p_, vsw], [PB[5]],
                                 skip_group_check=True)
                    if lvl is not None and lvl < 6:
                        continue
                    for bi, bk in ((1, 4), (2, 5)):
                        k.ts(den[:, bi * 4:bi * 4 + 4], psum[:, bk, 0:260].rearrange("p (h e) -> p h e", e=65)[:, :, 64], 1e-30, None,
                             ALU.max, None, [PB[bk]], [den], multi=True)
                    k.op("dve", lambda: nc.vector.reciprocal(coef[:, 4:12], den[:, 4:12]), [den], [coef], multi=True)
                    gv_ = gates_r[:, i, :].rearrange("p (h b) -> p b h", b=3)
                    k.tt(coef[:, :].rearrange("p (b h) -> p b h", h=4), coef[:, :].rearrange("p (b h) -> p b h", h=4), gv_, ALU.mult,
                         [coef, gates_r], [coef])
                    for h_ in range(4):
                        oh = onsa[:, h_ * 64:(h_ + 1) * 64]
                        k.ts(oh, acc_c[:, h_, :], coef[:, h_:h_ + 1], None, ALU.mult, None, [acc_c, coef], [onsa], multi=True)
                        k.stt(oh, psum[:, 4, h_ * 65:h_ * 65 + 64], coef[:, 4 + h_:5 + h_], oh, ALU.mult, ALU.add, [PB[4], coef, onsa], [onsa], multi=True)
                        k.stt(oh, psum[:, 5, h_ * 65:h_ * 65 + 64], coef[:, 8 + h_:9 + h_], oh, ALU.mult, ALU.add, [PB[5], coef, onsa], [onsa], multi=True)
                    ot = onT[i % 2]
                    for cc in range(2):
                        k.tr(psum[:, 6, cc * 128:(cc + 1) * 128], onsa[:, cc * 128:(cc + 1) * 128], ident_f[:, :], [onsa, ident_f], [PB[6]])
                    k.cp(ot[:, :, :], psum[:, 6, 0:256].rearrange("p (c q) -> p c q", c=2), [PB[6]], [ot], eng="act")
                    k.dma("sp", oT[0].rearrange("(c p) s -> p c s", p=128)[:, :, q0:q0 + 128], ot[:, :, :], [ot], [oT])
            k.barrier()
            if stop == "B2":
                break

            with contextlib.ExitStack() as pf:
                def psb(name, shape, dt=F32):
                    return Buf(pf.enter_context(nc.sbuf_tensor(name + "_%d" % l, list(shape), dt)))
                qa = [psb("qa%d" % j, [70, S], BF16) for j in range(2)]
                ka = [psb("ka%d" % j, [70, S], BF16) for j in range(2)]
                vf = psb("vf", [128, NT, 4, 65], BF16)
                for t_ in range(NT):
                    k.dma("sp", vf[:, t_, :, :], vaug[t_ * 128:(t_ + 1) * 128, 2:6, :], [vaug], [vf])
                pF = [psb("pF%d" % j, [128, 512], BF16) for j in range(3)]
                otok = psb("otok", [128, NT, 256])
                rden = psb("rden", [128, 4])
                ofT = [psb("ofT%d" % j, [128, 2, 128], BF16) for j in range(2)]
                npt = 0
                for h_ in range(4):
                    q_, k_ = qa[h_ % 2], ka[h_ % 2]
                    k.dma("sp", q_[:, :], fqa[h_, :, :], [fqa], [q_])
                    k.dma("sp", k_[:, :], fka[h_, :, :], [fka], [k_])
                    for g in range(NG):
                        ab = 4 + (g % 2)
                        k.mm(pb(ab, 260), zeros_b[0:1, 0:128], zeros_b[0:1, 0:260], True, False, [zeros_b], [PB[ab]], skip_group_check=True)
                        for kt in range(4 * (g + 1)):
                            j = kt - 4 * g
                            c0 = max(j, 0) * 128
                            sbk = npt % 4
                            k.mm(psum[:, sbk, c0:512], k_[:, kt * 128:(kt + 1) * 128], q_[:, g * 512 + c0:(g + 1) * 512], True, True,
                                 [k_, q_], [PB[sbk]])
                            if j >= 0:
                                k.tt(psum[:, sbk, c0:c0 + 128], psum[:, sbk, c0:c0 + 128], m_causal[:, :], ALU.add, [PB[sbk], m_causal], [PB[sbk]])
                            p_ = pF[npt % 3]
                            npt += 1
                            k.act(p_[:, c0:512], psum[:, sbk, c0:512], AF.Exp, [PB[sbk]], [p_])
                            for qs in range(max(j, 0), 4):
                                k.mm(psum[:, ab, qs * 65:qs * 65 + 65], p_[:, qs * 128:(qs + 1) * 128], vf[:, kt, h_, :], False, False,
                                     [p_, vf], [PB[ab]], skip_group_check=True)
                        k.op("dve", lambda: nc.vector.reciprocal(rden[:, :], psum[:, ab, 0:260].rearrange("p (q e) -> p q e", e=65)[:, :, 64]),
                             [PB[ab]], [rden])
                        for qs in range(4):
                            k.ts(otok[:, g * 4 + qs, h_ * 64:(h_ + 1) * 64], psum[:, ab, qs * 65:qs * 65 + 64], rden[:, qs:qs + 1], None,
                                 ALU.mult, None, [PB[ab], rden], [otok], multi=True)
                for tt_ in range(NT):
                    ot = ofT[tt_ % 2]
                    bk = 6 + tt_ % 2
                    for cc in range(2):
                        k.tr(psum[:, bk, cc * 128:(cc + 1) * 128], otok[:, tt_, cc * 128:(cc + 1) * 128], ident_f[:, :], [otok, ident_f], [PB[bk]])
                    k.cp(ot[:, :, :], psum[:, bk, 0:256].rearrange("p (c q) -> p c q", c=2), [PB[bk]], [ot], eng="act")
                    k.dma("sp", oT[1].rearrange("(c p) s -> p c s", p=128)[:, :, tt_ * 128:(tt_ + 1) * 128], ot[:, :, :], [ot], [oT])
            k.barrier()
            if stop == "B3":
                break

            with contextlib.ExitStack() as pg:
                def psb(name, shape, dt=F32):
                    return Buf(pg.enter_context(nc.sbuf_tensor(name + "_%d" % l, list(shape), dt)))
                wsf = psb("wsf", [128, 4, 128])
                wsb = psb("wsb", [128, 4, 128], BF16)
                tril = psb("tril", [128, 128])
                bsf = psb("bsf", [1, 512])
                bsh = psb("bsh", [1, 512], BF16)
                bsl = psb("bsl", [1, 512], BF16)
                k.dma("sp", wsf[:, :, :], g_wsT.h[l], [g_wsT], [wsf])
                k.dma("sp", tril[:, :], cin["m_tril"][:, :], [cin["m_tril"]], [tril])
                k.dma("sp", bsf[:, :], g_bs.h[l], [g_bs], [bsf])
                k.tt(wsb[:, :, :], wsf[:, :, :], tril[:, :].unsqueeze(1).to_broadcast([128, 4, 128]), ALU.mult, [wsf, tril], [wsb])
                k.cp(bsh[:, :], bsf[:, :], [bsf], [bsh])
                k.tt(bsf[:, :], bsf[:, :], bsh[:, :], ALU.subtract, [bsf, bsh], [bsf])
                k.cp(bsl[:, :], bsf[:, :], [bsf], [bsl])
                gvt = [psb("gvt%d" % j, [128, 256], BF16) for j in range(2)]
                gut = [psb("gut%d" % j, [128, 2, 128], BF16) for j in range(2)]
                ogt = [psb("ogt%d" % j, [128, 2, 128], BF16) for j in range(2)]
                for ch in range(NT):
                    gv_, gu_, og_ = gvt[ch % 2], gut[ch % 2], ogt[ch % 2]
                    k.dma("sp", gv_[:, :], gvn[ch * 128:(ch + 1) * 128, :], [gvn], [gv_])
                    k.dma("sp", gu_[:, :, :], guT.h.rearrange("(c p) s -> p c s", p=128)[:, :, ch * 128:(ch + 1) * 128], [guT], [gu_])
                    bk = ch % 2
                    for gg in range(4):
                        o_ = psum[(gg % 2) * 64:(gg % 2) * 64 + 64, bk, (gg // 2) * 128:(gg // 2) * 128 + 128]
                        k.mm(o_, gv_[:, gg * 64:(gg + 1) * 64], wsb[:, gg, :], True, False, [gv_, wsb], [PB[bk]], skip_group_check=True)
                        k.mm(o_, ones_b[0:1, 0:64], bsh[0:1, gg * 128:(gg + 1) * 128], False, False, [ones_b, bsh], [PB[bk]], skip_group_check=True)
                        k.mm(o_, ones_b[0:1, 0:64], bsl[0:1, gg * 128:(gg + 1) * 128], False, True, [ones_b, bsl], [PB[bk]], skip_group_check=True)
                    k.tt(og_[:, :, :], psum[:, bk, 0:256].rearrange("p (c t) -> p c t", c=2), gu_[:, :, :], ALU.mult, [PB[bk], gu_], [og_])
                    k.dma("sp", oT[2].rearrange("(c p) s -> p c s", p=128)[:, :, ch * 128:(ch + 1) * 128], og_[:, :, :], [og_], [oT])
            k.barrier()
            with contextlib.ExitStack() as pcv:
                def psb(name, shape, dt=F32):
                    return Buf(pcv.enter_context(nc.sbuf_tensor(name + "_%d" % l, list(shape), dt)))
                hin = [psb("hin%d" % j, [128, 32 + S]) for j in range(2)]
                cac = [psb("cac%d" % j, [128, S]) for j in range(2)]
                cwt = [psb("cwt%d" % j, [128, 31]) for j in range(2)]
                cpr = [psb("cpr%d" % j, [128, 3]) for j in range(2)]
                scr = {"sq": psb("c_sq", [128, 512]), "mean": psb("c_mean", [128, 512]), "rstd": psb("c_rstd", [128, 512])}
                cy = [psb("cy%d" % j, [128, 512]) for j in range(2)]
                cyb = [psb("cyb%d" % j, [128, 512], BF16) for j in range(2)]
                for ct in range(2):
                    k.dma("sp", hin[ct][:, :], hcT[ct * 128:(ct + 1) * 128, :], [hcT], [hin[ct]])
                    k.dma("sp", cwt[ct][:, :], c_wT.h[l][ct * 128:(ct + 1) * 128, :], [c_wT], [cwt[ct]])
                    k.dma("sp", cpr[ct][:, :], c_par.h[l][ct * 128:(ct + 1) * 128, :], [c_par], [cpr[ct]])
                    for g in range(NG):
                        a_ = cac[ct][:, g * 512:(g + 1) * 512]
                        k.ts(a_, hin[ct][:, 2 + g * 512:2 + g * 512 + 512], cwt[ct][:, 0:1], cpr[ct][:, 0:1], ALU.mult, ALU.add,
                             [hin[ct], cwt[ct], cpr[ct]], [cac[ct]], multi=True)
                        for j in range(1, 31):
                            k.stt(a_, hin[ct][:, 2 + j + g * 512:2 + j + g * 512 + 512], cwt[ct][:, j:j + 1], a_, ALU.mult, ALU.add,
                                  [hin[ct], cwt[ct]], [cac[ct]], multi=True)
                cnt5 = [0]
                for g in range(NG):
                    xs = [(cac[ct][:, g * 512:(g + 1) * 512], cac[ct]) for ct in range(2)]

                    def outf(i_, xa, xb, mean, rstd, Bm, Br, g=g):
                        y = cy[cnt5[0] % 2]
                        yb = cyb[cnt5[0] % 2]
                        cnt5[0] += 1
                        k.tt(y[:, :], xa, mean, ALU.subtract, [xb, Bm], [y])
                        k.tt(y[:, :], y[:, :], rstd, ALU.mult, [y, Br], [y])
                        k.ts(y[:, :], y[:, :], cpr[i_][:, 1:2], cpr[i_][:, 2:3], ALU.mult, ALU.add, [y, cpr[i_]], [y])
                        k.act(yb[:, :], y[:, :], AF.Silu, [y], [yb])
                        k.dma("sp", oT[3, i_ * 128:(i_ + 1) * 128, g * 512:(g + 1) * 512], yb[:, :], [yb], [oT])
                    ln_featmajor(xs, 512, onesC, scr, 0 + 2 * (g % 2), 1 + 2 * (g % 2), outf)
            k.barrier()
            if stop == "B5":
                break

            with contextlib.ExitStack() as pd:
                def psb(name, shape, dt=F32):
                    return Buf(pd.enter_context(nc.sbuf_tensor(name + "_%d" % l, list(shape), dt)))
                wg = [psb("wg%d" % j, [128, 8, 4, 128], BF16) for j in range(2)]
                bg = [psb("bg%d" % j, [1, 4, 128], BF16) for j in range(2)]
                wb = [psb("wb%d" % j, [128, 4, 2, 128], BF16) for j in range(2)]
                ob = [psb("ob%d" % j, [128, 4, 2, 512], BF16) for j in range(2)]
                sgt = [psb("sgt%d" % j, [128, 512], BF16) for j in range(2)]
                macc = [psb("macc%d" % j, [128, 512]) for j in range(2)]
                mtmp = [psb("mtmp%d" % j, [128, 512]) for j in range(2)]
                mob = [psb("mob%d" % j, [128, 512], BF16) for j in range(2)]
                wi = w_in.h[l].rearrange("(c p) n -> p c n", p=128)
                it = 0
                for dt_ in range(8):
                    w_, b_, wb_ = wg[dt_ % 2], bg[dt_ % 2], wb[dt_ % 2]
                    for n in range(4):
                        c0 = O_MG + n * D + dt_ * 128
                        k.dma("pool", w_[:, :, n, :], wi[:, :, c0:c0 + 128], [w_in], [w_])
                        k.dma("pool", b_[0:1, n, :], b_in.h[l][0:1, c0:c0 + 128], [b_in], [b_])
                        k.dma("pool", wb_[:, n, :, :], w_br.h[l, n].rearrange("(c p) d -> p c d", p=128)[:, :, dt_ * 128:(dt_ + 1) * 128],
                              [w_br], [wb_])
                    for g in range(NG):
                        o_ = ob[it % 2]
                        k.dma("sp", o_[:, :, :, :], oT.h.rearrange("n (c p) s -> p n c s", p=128)[:, :, :, g * 512:(g + 1) * 512], [oT], [o_])
                        ma = macc[it % 2]
                        for n in range(4):
                            bA = (n % 2) * 2 + (it % 2) * 4
                            bB = bA + 1
                            for c in range(8):
                                k.mm(pb(bA), w_[:, c, n, :], xT_bf[:, c, g * 512:(g + 1) * 512], c == 0, False, [w_, xT_bf], [PB[bA]])
                            k.mm(pb(bA), b_[0:1, n, :], ones_b[0:1, 0:512], False, True, [b_, ones_b], [PB[bA]])
                            for c in range(2):
                                k.mm(pb(bB), wb_[:, n, c, :], o_[:, n, c, :], c == 0, c == 1, [wb_, o_], [PB[bB]])
                            sg_ = sgt[n % 2]
                            k.act(sg_[:, :], pb(bA), AF.Sigmoid, [PB[bA]], [sg_])
                            if n == 0:
                                k.tt(ma[:, :], pb(bB), sg_[:, :], ALU.mult, [PB[bB], sg_], [ma])
                            else:
                                mt = mtmp[n % 2]
                                k.tt(mt[:, :], pb(bB), sg_[:, :], ALU.mult, [PB[bB], sg_], [mt])
                                if n < 3:
                                    k.tt(ma[:, :], ma[:, :], mt[:, :], ALU.add, [ma, mt], [ma], eng="pool")
                                else:
                                    mo = mob[it % 2]
                                    k.tt(mo[:, :], ma[:, :], mt[:, :], ALU.add, [ma, mt], [mo], eng="pool")
                                    k.dma("sp", mixT[dt_ * 128:(dt_ + 1) * 128, g * 512:(g + 1) * 512], mo[:, :], [mo], [mixT])
                        it += 1
            k.barrier()
            if stop == "D1":
                break

            with contextlib.ExitStack() as pm:
                def msb(name, shape, dt=F32):
                    return Buf(pm.enter_context(nc.sbuf_tensor(name + "_%d" % l, list(shape), dt)))
                wts_all = msb("wts_all", [128, NT, 4])
                dest_all = msb("dest_all", [128, NT, 4], I32)
                scr = {"sq": msb("l_sq", [128, 512]), "mean": msb("l_mean", [128, 512]), "rstd": msb("l_rstd", [128, 512])}

                def resid_ln(xr, lnp, g, final_dst):
                    xs = [(xr[:, c, :], xr) for c in range(8)]

                    def outf(i_, xa, xb, mean, rstd, Bm, Br):
                        k.tt(xa, xa, mean, ALU.subtract, [xb, Bm], [xb], multi=True)
                        k.tt(xa, xa, rstd, ALU.mult, [xb, Br], [xb], multi=True)
                        k.ts(xa, xa, lnp[:, i_, 0:1], lnp[:, i_, 1:2], ALU.mult, ALU.add, [xb, lnp], [xb], multi=True)
                        k.cp(xT_bf[:, i_, g * 512:(g + 1) * 512], xa, [xb], [xT_bf], eng="act", multi=True)
                        k.dma("sp", final_dst[i_ * 128:(i_ + 1) * 128, g * 512:(g + 1) * 512], xa, [xb], [final_dst])
                    ln_featmajor(xs, 512, onesD, scr, 6, 7, outf)

                with contextlib.ExitStack() as pd2:
                    def psb(name, shape, dt=F32):
                        return Buf(pd2.enter_context(nc.sbuf_tensor(name + "_%d" % l, list(shape), dt)))
                    wo = psb("wo", [128, 8, D], BF16)
                    for c in range(8):
                        k.dma("pool", wo[:, c, :], w_o.h[l][c * 128:(c + 1) * 128, :], [w_o], [wo])
                    ln1p = psb("ln1p", [128, 8, 2])
                    k.dma("sp", ln1p[:, :, :], ln1.h[l], [ln1], [ln1p])
                    wr = psb("wr", [128, 8, NE])
                    brr = psb("brr", [1, NE])
                    k.dma("sp", wr[:, :, :], w_r.h[l].rearrange("(c p) e -> p c e", p=128), [w_r], [wr])
                    k.dma("sp", brr[:, :], b_r.h[l], [b_r], [brr])
                    ustr = psb("ustr", [128, 128], BF16)
                    k.dma("pool", ustr[:, :], cin["u_strict"][:, :], [cin["u_strict"]], [ustr])
                    iota = psb("iota", [128, 32])
                    k.dma("sp", iota[:, :], cin["iota32"][:, :], [cin["iota32"]], [iota])
                    mcum = psb("mcum", [128, NE], BF16)
                    k.memset(mcum[:, :], 0.0, [mcum])
                    mp = [psb("mp%d" % j, [128, 8, 512], BF16) for j in range(2)]
                    xr = [psb("xr%d" % j, [128, 8, 512]) for j in range(2)]
                    lg = psb("lg", [128, NE])
                    mx8 = psb("mx8", [128, 8])
                    ix8 = psb("ix8", [128, 8], U32)
                    ixf = psb("ixf", [128, 4])
                    nmx = psb("nmx", [128, 1])
                    ew = psb("ew", [128, 4])
                    esum = psb("esum", [128, 1])
                    mskf = psb("mskf", [128, NE])
                    mskb = psb("mskb", [128, NE], BF16)
                    oh = psb("oh", [128, NE])
                    ohj = psb("ohj", [128, NE])
                    posk = psb("posk", [128, 4])
                    dstf = psb("dstf", [128, 4])
                    xtok = [psb("xtok%d" % j, [128, D], BF16) for j in range(2)]
                    for g in range(NG):
                        m_, x_ = mp[g % 2], xr[g % 2]
                        k.dma("sp", m_[:, :, :], mixT.h.rearrange("(c p) s -> p c s", p=128)[:, :, g * 512:(g + 1) * 512], [mixT], [m_])
                        k.dma("sp", x_[:, :, :], xres.h.rearrange("(c p) s -> p c s", p=128)[:, :, g * 512:(g + 1) * 512], [xres], [x_])
                        for dd in range(8):
                            bk = dd % 4
                            for c in range(8):
                                k.mm(pb(bk), wo[:, c, dd * 128:(dd + 1) * 128], m_[:, c, :], c == 0, c == 7, [wo, m_], [PB[bk]])
                            k.stt(x_[:, dd, :], x_[:, dd, :], ALPHA, pb(bk), ALU.mult, ALU.add, [x_, PB[bk]], [x_], multi=True)
                        resid_ln(x_, ln1p, g, xres)
                        for t4 in range(4):
                            tt_ = g * 4 + t4
                            for c in range(8):
                                k.mm(pb(4, NE), x_[:, c, t4 * 128:(t4 + 1) * 128], wr[:, c, :], c == 0, False, [x_, wr], [PB[4]])
                            k.mm(pb(4, NE), ones_f[0:1, 0:128], brr[0:1, :], False, True, [ones_f, brr], [PB[4]])
                            k.cp(lg[:, :], pb(4, NE), [PB[4]], [lg])
                            k.op("dve", lambda: nc.vector.max(out=mx8[:, :], in_=lg[:, :]), [lg], [mx8])
                            k.op("dve", lambda: nc.vector.max_index(out=ix8[:, :], in_max=mx8[:, :], in_values=lg[:, :]), [mx8, lg], [ix8])
                            k.ts(nmx[:, :], mx8[:, 0:1], -1.0, None, ALU.mult, None, [mx8], [nmx])
                            k.act(ew[:, :], mx8[:, 0:4], AF.Exp, [mx8, nmx], [ew], bias=nmx[:, 0:1])
                            k.op("dve", lambda: nc.vector.reduce_sum(out=esum[:, :], in_=ew[:, :], axis=mybir.AxisListType.X), [ew], [esum])
                            k.op("dve", lambda: nc.vector.reciprocal(esum[:, :], esum[:, :]), [esum], [esum])
                            k.ts(wts_all[:, tt_, :], ew[:, :], esum[:, 0:1], None, ALU.mult, None, [ew, esum], [wts_all], multi=True)
                            k.ts(mskf[:, :], lg[:, :], mx8[:, 3:4], None, ALU.is_ge, None, [lg, mx8], [mskf])
                            k.cp(mskb[:, :], mskf[:, :], [mskf], [mskb])
                            k.mm(pb(5, NE), ustr[:, :], mskb[:, :], True, False, [ustr, mskb], [PB[5]])
                            k.mm(pb(5, NE), ones_b[:, 0:128], mcum[:, :], False, True, [ones_b, mcum], [PB[5]])
                            k.cp(ixf[:, :], ix8[:, 0:4], [ix8], [ixf])
                            for kk in range(4):
                                k.ts(oh[:, :], iota[:, :], ixf[:, kk:kk + 1], None, ALU.is_equal, None, [iota, ixf], [oh])
                                k.tt(ohj[:, :], oh[:, :], pb(5, NE), ALU.mult, [oh, PB[5]], [ohj])
                                k.op("dve", lambda: nc.vector.reduce_sum(out=posk[:, kk:kk + 1], in_=ohj[:, :], axis=mybir.AxisListType.X),
                                     [ohj], [posk], multi=True)
                            k.tt(mcum[:, :], mcum[:, :], mskb[:, :], ALU.add, [mcum, mskb], [mcum])
                            k.ts(posk[:, :], posk[:, :], float(CAP - 1), None, ALU.min, None, [posk], [posk])
                            k.stt(dstf[:, :], ixf[:, :], float(CAP), posk[:, :], ALU.mult, ALU.add, [ixf, posk], [dstf])
                            k.cp(dest_all[:, tt_, :], dstf[:, :], [dstf], [dest_all], multi=True)
                            xt_ = xtok[tt_ % 2]
                            bk = 2 + tt_ % 2
                            for c in range(8):
                                k.tr(pbb(bk)[:, c * 128:(c + 1) * 128], xT_bf[:, c, tt_ * 128:(tt_ + 1) * 128], ident_b[:, :], [xT_bf, ident_b], [PB[bk]])
                            k.cp(xt_[:, :], pbb(bk)[:, :], [PB[bk]], [xt_], eng="act")
                            for kk in range(4):
                                k.dma("pool", xe_d[:, :], xt_[:, :], [xt_, dest_all], [xe_d],
                                      indirect=dict(out_offset=bass.IndirectOffsetOnAxis(ap=dest_all[:, tt_, kk:kk + 1], axis=0), in_offset=None))
                k.barrier()
                if stop == "D2":
                    break

                with contextlib.ExitStack() as pe_:
                    def psb(name, shape, dt=F32):
                        return Buf(pe_.enter_context(nc.sbuf_tensor(name + "_%d" % l, list(shape), dt)))
                    xel = [psb("xel%d" % j, [128, D], BF16) for j in range(3)]
                    xeT = [psb("xeT%d" % j, [128, 8, CAP], BF16) for j in range(2)]
                    w1s = [psb("w1s%d" % j, [128, 8, 2, 128], BF16) for j in range(3)]
                    b1s = [psb("b1s%d" % j, [1, 2, D], BF16) for j in range(2)]
                    w2s = [psb("w2s%d" % j, [128, 8, D], BF16) for j in range(2)]
                    b2s = [psb("b2s%d" % j, [1, D], BF16) for j in range(2)]
                    aT = [psb("aT%d" % j, [128, 8, CAP], BF16) for j in range(2)]
                    eg = [psb("eg%d" % j, [128, 320]) for j in range(2)]
                    el = [psb("el%d" % j, [128, 320]) for j in range(2)]
                    esg = [psb("esg%d" % j, [128, 320]) for j in range(2)]
                    yst = [psb("yst%d" % j, [128, D], BF16) for j in range(2)]
                    nx = 0
                    nw = 0
                    ne_ = 0
                    HS = CAP // 2
                    for e in range(NE):
                        xT_ = xeT[e % 2]
                        a_ = aT[e % 2]
                        for st in range(CAP // 128):
                            xl = xel[nx % 3]
                            bk = nx % 2
                            nx += 1
                            k.dma("sp", xl[:, :], xe_d[e * CAP + st * 128:e * CAP + (st + 1) * 128, :], [xe_d], [xl])
                            for c in range(8):
                                k.tr(pbb(bk)[:, c * 128:(c + 1) * 128], xl[:, c * 128:(c + 1) * 128], ident_b[:, :], [xl, ident_b], [PB[bk]])
                            k.cp(xT_[:, :, st * 128:(st + 1) * 128], pbb(bk)[:, :].rearrange("p (c s) -> p c s", c=8), [PB[bk]], [xT_],
                                 eng="act", multi=True)
                        b1_ = b1s[e % 2]
                        k.dma("pool", b1_[0:1, 0, :], b1g.h[l, e], [b1g], [b1_])
                        k.dma("pool", b1_[0:1, 1, :], b1l.h[l, e], [b1l], [b1_])
                        w2_ = w2s[e % 2]
                        b2_ = b2s[e % 2]
                        for c in range(8):
                            k.dma("pool", w2_[:, c, :], w2.h[l, e][c * 128:(c + 1) * 128, :], [w2], [w2_])
                        k.dma("pool", b2_[0:1, :], b2.h[l, e], [b2], [b2_])
                        for jt in range(8):
                            w1_ = w1s[nw % 3]
                            nw += 1
                            k.dma("pool", w1_[:, :, 0, :], w1g.h[l, e].rearrange("(c p) n -> p c n", p=128)[:, :, jt * 128:(jt + 1) * 128], [w1g], [w1_])
                            k.dma("pool", w1_[:, :, 1, :], w1l.h[l, e].rearrange("(c p) n -> p c n", p=128)[:, :, jt * 128:(jt + 1) * 128], [w1l], [w1_])
                            for hf in range(2):
                                bG = 2 + (ne_ % 2) * 2
                                bL = bG + 1
                                for (bk, wi_) in ((bG, 0), (bL, 1)):
                                    for c in range(8):
                                        k.mm(pb(bk, HS), w1_[:, c, wi_, :], xT_[:, c, hf * HS:(hf + 1) * HS], c == 0, False, [w1_, xT_], [PB[bk]])
                                    k.mm(pb(bk, HS), b1_[0:1, wi_, jt * 128:(jt + 1) * 128], ones_b[0:1, 0:HS], False, True, [b1_, ones_b], [PB[bk]])
                                g_, l_, s_ = eg[ne_ % 2], el[ne_ % 2], esg[ne_ % 2]
                                ne_ += 1
                                k.ts(g_[:, :], pb(bG, HS), 7.0, None, ALU.min, None, [PB[bG]], [g_])
                                k.act(s_[:, :], g_[:, :], AF.Sigmoid, [g_], [s_], scale=1.702)
                                k.ts(l_[:, :], pb(bL, HS), 7.0, -7.0, ALU.min, ALU.max, [PB[bL]], [l_])
                                k.stt(l_[:, :], l_[:, :], 1.0, g_[:, :], ALU.add, ALU.mult, [l_, g_], [l_])
                                k.tt(a_[:, jt, hf * HS:(hf + 1) * HS], l_[:, :], s_[:, :], ALU.mult, [l_, s_], [a_], multi=True)
                        for st in range(CAP // 128):
                            ys = yst[st % 2]
                            for hf in range(2):
                                bk = 6 + hf
                                for jt in range(8):
                                    k.mm(pb(bk), a_[:, jt, st * 128:(st + 1) * 128], w2_[:, jt, hf * 512:(hf + 1) * 512], jt == 0, False, [a_, w2_], [PB[bk]])
                                k.mm(pb(bk), ones_b[0:1, 0:128], b2_[0:1, hf * 512:(hf + 1) * 512], False, True, [ones_b, b2_], [PB[bk]])
                                k.cp(ys[:, hf * 512:(hf + 1) * 512], pb(bk), [PB[bk]], [ys], eng="act", multi=True)
                            k.dma("sp", yr_d[e * CAP + st * 128:e * CAP + (st + 1) * 128, :], ys[:, :], [ys], [yr_d])
                k.barrier()
                if stop == "E":
                    break

                with contextlib.ExitStack() as pz:
                    def psb(name, shape, dt=F32):
                        return Buf(pz.enter_context(nc.sbuf_tensor(name + "_%d" % l, list(shape), dt)))
                    ln2p = psb("ln2p", [128, 8, 2])
                    k.dma("sp", ln2p[:, :, :], ln2.h[l], [ln2], [ln2p])
                    gk = [psb("gk%d" % j, [128, D], BF16) for j in range(4)]
                    ytok = [psb("ytok%d" % j, [128, D]) for j in range(2)]
                    xr = [psb("xr2_%d" % j, [128, 8, 512]) for j in range(2)]
                    for g in range(NG):
                        x_ = xr[g % 2]
                        k.dma("sp", x_[:, :, :], xres.h.rearrange("(c p) s -> p c s", p=128)[:, :, g * 512:(g + 1) * 512], [xres], [x_])
                        for t4 in range(4):
                            tt_ = g * 4 + t4
                            y_ = ytok[tt_ % 2]
                            for kk in range(4):
                                k.dma("pool", gk[kk][:, :], yr_d[:, :], [yr_d, dest_all], [gk[kk]], multi=False,
                                      indirect=dict(in_offset=bass.IndirectOffsetOnAxis(ap=dest_all[:, tt_, kk:kk + 1], axis=0), out_offset=None))
                                if kk == 0:
                                    k.ts(y_[:, :], gk[kk][:, :], wts_all[:, tt_, 0:1], None, ALU.mult, None, [gk[kk], wts_all], [y_])
                                else:
                                    k.stt(y_[:, :], gk[kk][:, :], wts_all[:, tt_, kk:kk + 1], y_[:, :], ALU.mult, ALU.add, [gk[kk], wts_all, y_], [y_])
                            for hf in range(2):
                                bk = (tt_ % 2) * 2 + hf
                                for c in range(4):
                                    cc = hf * 4 + c
                                    k.tr(psum[:, bk, c * 128:(c + 1) * 128], y_[:, cc * 128:(cc + 1) * 128], ident_f[:, :], [y_, ident_f], [PB[bk]])
                                xv = x_[:, hf * 4:(hf + 1) * 4, t4 * 128:(t4 + 1) * 128]
                                k.stt(xv, xv, ALPHA, psum[:, bk, :].rearrange("p (c t) -> p c t", c=4), ALU.mult, ALU.add, [x_, PB[bk]], [x_], multi=True)
                        resid_ln(x_, ln2p, g, yT_out if last else xres)
                k.barrier()
        k.barrier()
    return nc, k, dbg


def _bf(x):
    return np.ascontiguousarray(x, dtype=np.float32)


def make_inputs(inp, b):
    f = lambda a: np.ascontiguousarray(np.asarray(a), dtype=np.float32)
    w_in = f(inp["w_in"])
    b_in = f(inp["b_in"])
    def swap_cols(w, c0, nh):
        out = []
        for h in range(nh):
            blk = w[..., c0 + h * 64:c0 + (h + 1) * 64]
            out.append(np.concatenate([blk[..., 8:16], blk[..., 0:8], blk[..., 16:64]], axis=-1))
        return np.concatenate(out, axis=-1)
    w_sw = np.concatenate([swap_cols(w_in, O_NQ, 4), swap_cols(w_in, O_NKC, 1), swap_cols(w_in, O_NKS, 1), swap_cols(w_in, O_NKW, 1)], axis=-1)
    b_sw = np.concatenate([swap_cols(b_in, O_NQ, 4), swap_cols(b_in, O_NKC, 1), swap_cols(b_in, O_NKS, 1), swap_cols(b_in, O_NKW, 1)], axis=-1)
    m = {}
    m["xT"] = np.ascontiguousarray(f(inp["x"])[b].T)
    m["pos"] = np.ascontiguousarray(np.asarray(inp["positions"])[b:b + 1].astype(np.int32))
    m["w_in"] = w_in
    m["b_in"] = b_in[:, None, :]
    m["w_sw"] = np.ascontiguousarray(w_sw)
    m["b_sw"] = np.ascontiguousarray(b_sw[:, None, :])
    m["pe_kT"] = np.ascontiguousarray(f(inp["nsa_pe_k"]).transpose(0, 2, 1))
    m["pe_vT"] = np.ascontiguousarray(f(inp["nsa_pe_v"]).transpose(0, 2, 1))
    m["cw1k"] = f(inp["nsa_cmp_w1_k"])
    m["cw2k"] = f(inp["nsa_cmp_w2_k"])
    m["cw1v"] = f(inp["nsa_cmp_w1_v"])
    m["cw2v"] = f(inp["nsa_cmp_w2_v"])
    m["g_lng"] = f(inp["gmlp_ln_g"])[:, None, :]
    m["g_lnb"] = f(inp["gmlp_ln_b"])[:, None, :]
    m["g_wsT"] = np.ascontiguousarray(f(inp["gmlp_w_s"]).transpose(0, 3, 1, 2))
    m["g_bs"] = f(inp["gmlp_b_s"]).reshape(L, 1, 512)
    m["c_wT"] = np.ascontiguousarray(f(inp["conv_w"])[:, :, 0, :].transpose(0, 2, 1))
    m["c_par"] = np.ascontiguousarray(np.stack([f(inp["conv_b"]), f(inp["conv_ln_g"]), f(inp["conv_ln_b"])], axis=-1))
    m["w_br"] = f(inp["w_br"])
    m["w_o"] = f(inp["w_o"])
    lay = lambda g_, b_: np.ascontiguousarray(np.stack([f(g_).reshape(L, 8, 128), f(b_).reshape(L, 8, 128)], axis=-1).transpose(0, 2, 1, 3))
    m["ln1"] = lay(inp["ln1_g"], inp["ln1_b"])
    m["ln2"] = lay(inp["ln2_g"], inp["ln2_b"])
    m["w_r"] = f(inp["w_router"])
    m["b_r"] = f(inp["b_router"])[:, None, :]
    w1 = f(inp["w_exp1"])
    m["w1g"] = np.ascontiguousarray(w1[..., 0::2])
    m["w1l"] = np.ascontiguousarray(w1[..., 1::2])
    b1 = f(inp["b_exp1"])
    m["b1g"] = np.ascontiguousarray(b1[..., 0::2])[:, :, None, :]
    m["b1l"] = np.ascontiguousarray(b1[..., 1::2])[:, :, None, :]
    m["w2"] = f(inp["w_exp2"])
    m["b2"] = f(inp["b_exp2"])[:, :, None, :]
    for n, v in host_consts().items():
        m["c_" + n] = v
    return m


_CACHE = {}


def kernel(**inputs):
    if "nc" not in _CACHE:
        _CACHE["nc"] = build()[0]
    nc = _CACHE["nc"]
    shared = make_inputs(inputs, 0)
    in_maps = []
    x = np.asarray(inputs["x"], dtype=np.float32)
    pos = np.asarray(inputs["positions"]).astype(np.int32)
    NCORES = 4
    for c in range(NCORES):
        b = c % 4
        m = dict(shared)
        m["xT"] = np.ascontiguousarray(x[b].T)
        m["pos"] = np.ascontiguousarray(pos[b:b + 1])
        in_maps.append(m)
    res = run_bass_kernel_spmd(nc, in_maps, core_ids=list(range(NCORES)))
    out = np.stack([np.ascontiguousarray(res.results[b]["yT"].T) for b in range(4)], axis=0)
    return out.astype(np.float32)
```

```python
import contextlib
import numpy as np
import concourse.bass as bass
import concourse.mybir as mybir
from concourse.bass_utils import run_bass_kernel_spmd

F32 = mybir.dt.float32
BF16 = mybir.dt.bfloat16
I32 = mybir.dt.int32
U32 = mybir.dt.uint32
AF = mybir.ActivationFunctionType
ALU = mybir.AluOpType

S = 4096
D = 1024
NT = S // 128
NG = S // 512
L = 4
NIN = 6544
CAP = 640
NE = 32
ALPHA = float((2 * L) ** 0.25)
EPS = 1e-5
NEG = -30000.0
BIG = 32768.0
O_NQ, O_NKC, O_NVC, O_NKS, O_NVS, O_NKW, O_NVW, O_NG, O_FQ, O_FK, O_FV, O_FF, O_GU, O_GV, O_CA, O_CB, O_MG = (
    0, 256, 320, 384, 448, 512, 576, 640, 652, 908, 1164, 1420, 1424, 1680, 1936, 2192, 2448)
SEM_MAX = 30000
DMA_MAX = 1800
SAME_SYNC = True


class Buf:
    def __init__(self, h=None):
        self.h = h
        self.ws = {}
        self.rs = {}
        self.prs = {}

    def __getitem__(self, idx):
        return self.h[idx]


class Eng:
    def __init__(self, name, e):
        self.name = name
        self.e = e
        self.sem = None
        self.cnt = 0
        self.known = {}


def _merge(d, s):
    for kk, v in s.items():
        if d.get(kk, 0) < v:
            d[kk] = v


class KB:
    def __init__(self, nc):
        self.nc = nc
        self.es = contextlib.ExitStack()
        self.E = {n: Eng(n, e) for n, e in (("pe", nc.tensor), ("act", nc.scalar), ("dve", nc.vector),
                                            ("pool", nc.gpsimd), ("sp", nc.sync))}
        self.sems = []
        self.semown = []
        self.lanes = {"sp": [], "pool": [], "act": []}
        self.lane_i = {"sp": 0, "pool": 0, "act": 0}
        self.nl = 6
        self.ninst = 0

    def new_sem(self, owner):
        s = self.es.enter_context(self.nc.semaphore("s%d" % len(self.sems)))
        self.sems.append(s)
        self.semown.append(owner)
        return len(self.sems) - 1

    def _wait(self, E, deps):
        for si, val in deps.items():
            if self.semown[si] == E.name and (E.name == "pe" or not SAME_SYNC):
                continue
            if E.known.get(si, 0) < val:
                E.e.wait_ge(self.sems[si], val)
                E.known[si] = val
                self.ninst += 1

    def _deps(self, R, W, multi):
        deps = {}
        for b in R:
            _merge(deps, b.ws)
        for b in W:
            if b.rs:
                b.prs = b.rs
                b.rs = {}
                b.ws = {}
            _merge(deps, b.prs)
            if not multi:
                _merge(deps, b.ws)
        return deps

    def _commit(self, R, W, multi, si, val):
        for b in R:
            if b.rs.get(si, 0) < val:
                b.rs[si] = val
        for b in W:
            if multi:
                if b.ws.get(si, 0) < val:
                    b.ws[si] = val
            else:
                b.ws = {si: val}

    def op(self, en, thunk, R=(), W=(), multi=False):
        E = self.E[en]
        P = []
        for b in list(R) + list(W):
            if getattr(b, "psum", False) and b not in P:
                P.append(b)
        R = [b for b in R if not getattr(b, "psum", False)]
        W = [b for b in W if not getattr(b, "psum", False)]
        deps = self._deps(R, W, multi)
        _merge(deps, self._deps([], P, False))
        self._wait(E, deps)
        if E.sem is None or E.cnt >= SEM_MAX:
            E.sem = self.new_sem(en)
            E.cnt = 0
        ins = thunk()
        E.cnt += 1
        ins.then_inc(self.sems[E.sem], 1)
        self.ninst += 1
        self._commit(R, W, multi, E.sem, E.cnt)
        self._commit([], P, False, E.sem, E.cnt)

    def dma(self, q, out, in_, R=(), W=(), multi=True, indirect=None, **kw):
        E = self.E[q]
        lanes = self.lanes[q]
        if len(lanes) < self.nl:
            lanes.append([self.new_sem("dma"), 0])
            ln = lanes[-1]
        else:
            ln = lanes[self.lane_i[q] % self.nl]
            self.lane_i[q] += 1
        deps = self._deps(R, W, multi)
        if ln[1] > 0:
            _merge(deps, {ln[0]: 16 * ln[1]})
        self._wait(E, deps)
        if ln[1] >= DMA_MAX:
            ln[0] = self.new_sem("dma")
            ln[1] = 0
        if indirect is not None:
            ins = E.e.indirect_dma_start(out=out, in_=in_, **indirect)
        else:
            ins = E.e.dma_start(out=out, in_=in_, **kw)
        ln[1] += 1
        ins.then_inc(self.sems[ln[0]], 16)
        self.ninst += 1
        self._commit(R, W, multi, ln[0], 16 * ln[1])

    def barrier(self):
        ev = {}
        for E in self.E.values():
            if E.sem is not None and E.cnt > 0:
                ev[E.sem] = E.cnt
        for q in self.lanes:
            for ln in self.lanes[q]:
                if ln[1] > 0:
                    ev[ln[0]] = 16 * ln[1]
        for E in self.E.values():
            for si, val in ev.items():
                if self.semown[si] == E.name and E.name != "dma":
                    continue
                if E.known.get(si, 0) < val:
                    E.e.wait_ge(self.sems[si], val)
                    E.known[si] = val

    def mm(self, out, lhsT, rhs, start, stop, R, W, **kw):
        self.op("pe", lambda: self.nc.tensor.matmul(out, lhsT=lhsT, rhs=rhs, start=start, stop=stop, **kw), R, W)

    def tr(self, out, in_, ident, R, W):
        self.op("pe", lambda: self.nc.tensor.transpose(out, in_, ident), R, W)

    def act(self, out, in_, func, R, W, multi=False, **kw):
        self.op("act", lambda: self.nc.scalar.activation(out=out, in_=in_, func=func, **kw), R, W, multi)

    def tt(self, out, in0, in1, op, R, W, eng="dve", multi=False):
        e = self.nc.vector if eng == "dve" else self.nc.gpsimd
        self.op(eng, lambda: e.tensor_tensor(out=out, in0=in0, in1=in1, op=op), R, W, multi)

    def ts(self, out, in0, s1, s2, op0, op1, R, W, eng="dve", multi=False, **kw):
        e = self.nc.vector if eng == "dve" else self.nc.gpsimd
        if op1 is None:
            self.op(eng, lambda: e.tensor_scalar(out=out, in0=in0, scalar1=s1, scalar2=None, op0=op0, **kw), R, W, multi)
        else:
            self.op(eng, lambda: e.tensor_scalar(out=out, in0=in0, scalar1=s1, scalar2=s2, op0=op0, op1=op1, **kw), R, W, multi)

    def stt(self, out, in0, scalar, in1, op0, op1, R, W, multi=False):
        self.op("dve", lambda: self.nc.vector.scalar_tensor_tensor(out=out, in0=in0, scalar=scalar, in1=in1,
                                                                   op0=op0, op1=op1), R, W, multi)

    def cp(self, out, in_, R, W, eng="dve", multi=False):
        if eng == "act":
            self.act(out, in_, AF.Copy, R, W, multi)
        else:
            e = self.nc.vector if eng == "dve" else self.nc.gpsimd
            self.op(eng, lambda: e.tensor_copy(out=out, in_=in_), R, W, multi)

    def memset(self, ap, val, W, eng="dve", multi=False):
        e = self.nc.vector if eng == "dve" else self.nc.gpsimd
        self.op(eng, lambda: e.memset(ap, val), (), W, multi)


CONST_SPECS = {}


def host_consts():
    c = {}
    c["ident"] = np.eye(128, dtype=np.float32)
    kq = np.arange(128)
    c["m_causal"] = np.where(kq[:, None] <= kq[None, :], 0.0, NEG).astype(np.float32)
    c["m_band"] = np.where(kq[:, None] > kq[None, :], 0.0, NEG).astype(np.float32)
    c["m_tril"] = (kq[:, None] <= kq[None, :]).astype(np.float32)
    c["u_strict"] = (kq[:, None] < kq[None, :]).astype(np.float32)
    ncmp = 255
    cs = np.arange(256) * 16
    ss = np.arange(64) * 64
    ov = ((cs[:, None] <= ss[None, :] + 63) & (cs[:, None] + 31 >= ss[None, :])).astype(np.float32)
    ov[255] = 0
    c["ovl"] = np.concatenate([np.ones((256, 1), np.float32), ov], axis=1).reshape(2, 128, 65).transpose(1, 0, 2).copy()
    cend = np.arange(256) * 16 + 31
    cm = np.zeros((NT, 128, 2, 128), np.float32)
    for i in range(NT):
        tq = i * 128 + np.arange(128)
        for ct in range(2):
            ce = cend[ct * 128:(ct + 1) * 128]
            ok = (ce[:, None] <= tq[None, :]) & ((np.arange(128) + ct * 128)[:, None] < ncmp)
            cm[i, :, ct, :] = np.where(ok, 0.0, NEG)
    c["cmask"] = cm
    st = np.zeros((NT, 128, 2, 64), np.float32)
    j = np.arange(64)
    for i in range(NT):
        tq = i * 128 + np.arange(128)
        cur = (tq // 64)[:, None]
        causal = j[None, :] <= cur
        forced = (j[None, :] == 0) | (causal & (j[None, :] > cur - 2))
        st[i, :, 0, :] = causal
        st[i, :, 1, :] = (causal.astype(np.float32) - 1.0) + forced * 1e9
    c["seltab"] = st
    eb = np.zeros((64, S), np.float32)
    eb[np.arange(S) // 64, np.arange(S)] = BIG
    c["ebig"] = eb
    half = 8
    invf = (500000.0 ** (-np.arange(half, dtype=np.float32) / half)).astype(np.float32)
    fr = np.zeros((128, 1), np.float32)
    sg = np.ones((128, 1), np.float32)
    for r in range(128):
        rr = r % 64
        if rr < 16:
            fr[r, 0] = invf[rr % 8]
            if rr < 8:
                sg[r, 0] = -1.0
    c["ropef"] = np.concatenate([fr, sg], axis=1)
    c["iota32"] = np.tile(np.arange(32, dtype=np.float32)[None, :], (128, 1))
    return c


def build(nlayers=L, debug=False, stop=None):
    LW = nlayers
    lvl = None
    if stop is not None and ':' in stop:
        stop, lvl = stop.split(':')
        lvl = float(lvl)
    NEW = NE if stop in (None, 'E') else 1
    nc = bass.Bass("TRN2", target_bir_lowering=False)
    k = KB(nc)
    dbg = {}

    def din(name, shape, dt=F32):
        return Buf(nc.dram_tensor(name, list(shape), dt, kind="ExternalInput").ap())

    def dscr(name, shape, dt):
        kind = "ExternalOutput" if debug else "Internal"
        b = Buf(nc.dram_tensor(name, list(shape), dt, kind=kind).ap())
        dbg[name] = b
        return b

    cons = host_consts()
    xT_in = din("xT", [D, S])
    pos_in = din("pos", [1, S], I32)
    w_in = din("w_in", [LW, D, NIN])
    b_in = din("b_in", [LW, 1, NIN])
    w_sw = din("w_sw", [LW, D, 448])
    b_sw = din("b_sw", [LW, 1, 448])
    pe_kT = din("pe_kT", [LW, 64, 32])
    pe_vT = din("pe_vT", [LW, 64, 32])
    cw1k = din("cw1k", [LW, 2048, 64])
    cw2k = din("cw2k", [LW, 64, 64])
    cw1v = din("cw1v", [LW, 2048, 64])
    cw2v = din("cw2v", [LW, 64, 64])
    g_lng = din("g_lng", [LW, 1, 256])
    g_lnb = din("g_lnb", [LW, 1, 256])
    g_wsT = din("g_wsT", [LW, 128, 4, 128])
    g_bs = din("g_bs", [LW, 1, 512])
    c_wT = din("c_wT", [LW, 256, 31])
    c_par = din("c_par", [LW, 256, 3])
    w_br = din("w_br", [LW, 4, 256, D])
    w_o = din("w_o", [LW, D, D])
    ln1 = din("ln1", [LW, 128, 8, 2])
    ln2 = din("ln2", [LW, 128, 8, 2])
    w_r = din("w_r", [LW, D, NE])
    b_r = din("b_r", [LW, 1, NE])
    w1g = din("w1g", [LW, NEW, D, D])
    w1l = din("w1l", [LW, NEW, D, D])
    b1g = din("b1g", [LW, NEW, 1, D])
    b1l = din("b1l", [LW, NEW, 1, D])
    w2 = din("w2", [LW, NEW, D, D])
    b2 = din("b2", [LW, NEW, 1, D])
    cin = {n: din("c_" + n, list(v.shape)) for n, v in cons.items()}
    yT_out = Buf(nc.dram_tensor("yT", [D, S], F32, kind="ExternalOutput").ap())

    xres = dscr("xres", [D, S], F32)
    cosd = dscr("cosd", [128, S], F32)
    sind = dscr("sind", [128, S], F32)
    qn = dscr("qn", [4, 64, S], BF16)
    kcT = dscr("kcT", [64, S], BF16)
    vcT = dscr("vcT", [64, S], BF16)
    ksT = dscr("ksT", [64, S], BF16)
    kwT = dscr("kwT", [64, S], BF16)
    vaug = dscr("vaug", [S, 6, 65], BF16)
    gvn = dscr("gvn", [S, 256], BF16)
    fqa = dscr("fqa", [4, 70, S], BF16)
    fka = dscr("fka", [4, 70, S], BF16)
    guT = dscr("guT", [256, S], BF16)
    hcT = dscr("hcT", [256, 32 + S], BF16)
    oT = dscr("oT", [4, 256, S], BF16)
    mixT = dscr("mixT", [D, S], BF16)
    xe_d = dscr("xe_d", [NE * CAP, D], BF16)
    yr_d = dscr("yr_d", [NE * CAP, D], BF16)

    es = k.es
    with es:
        def sb(name, shape, dt=F32):
            return Buf(es.enter_context(nc.sbuf_tensor(name, list(shape), dt)))

        psum = es.enter_context(nc.psum_tensor("psum", [128, 8, 512], F32))
        PB = [Buf(psum) for _ in range(8)]
        for b_ in PB:
            b_.psum = True

        def pb(i, n=512, p0=0, p1=128):
            return psum[p0:p1, i, 0:n]

        def pbb(i):
            return psum[:, i, :].bitcast(BF16)

        ident_f = sb("ident_f", [128, 128])
        ident_b = sb("ident_b", [128, 128], BF16)
        m_causal = sb("m_causal", [128, 128])
        m_band = sb("m_band", [128, 128])
        ones_b = sb("ones_b", [128, 512], BF16)
        zeros_b = sb("zeros_b", [1, 512], BF16)
        ones_f = sb("ones_f", [128, 512])
        onesD = sb("onesD", [128, 128])
        onesC = sb("onesC", [128, 128])
        ropef = sb("ropef", [128, 2])
        xT_bf = sb("xT_bf", [128, 8, S], BF16)
        gates_r = sb("gates_r", [128, NT, 12])
        k.dma("sp", ident_f[:, :], cin["ident"][:, :], [cin["ident"]], [ident_f])
        k.dma("sp", m_causal[:, :], cin["m_causal"][:, :], [], [m_causal])
        k.dma("sp", m_band[:, :], cin["m_band"][:, :], [], [m_band])
        k.dma("sp", ropef[:, :], cin["ropef"][:, :], [], [ropef])
        k.cp(ident_b[:, :], ident_f[:, :], [ident_f], [ident_b])
        k.memset(ones_b[:, :], 1.0, [ones_b])
        k.memset(zeros_b[:, :], 0.0, [zeros_b])
        k.memset(ones_f[:, :], 1.0, [ones_f])
        k.memset(onesD[:, :], 1.0 / D, [onesD])
        k.memset(onesC[:, :], 1.0 / 256, [onesC])
        for c in range(8):
            k.dma("sp", xres[c * 128:(c + 1) * 128, :], xT_in[c * 128:(c + 1) * 128, :], [xT_in], [xres])
            k.dma("pool", xT_bf[:, c, :], xT_in[c * 128:(c + 1) * 128, :], [xT_in], [xT_bf], max_dma_last_dim=4096)
        with contextlib.ExitStack() as ps_:
            zt = Buf(ps_.enter_context(nc.sbuf_tensor("zt", [128, 32], BF16)))
            k.memset(zt[:, :], 0.0, [zt])
            for ct in range(2):
                k.dma("sp", hcT[ct * 128:(ct + 1) * 128, 0:32], zt[:, :], [zt], [hcT])
            pi_ = Buf(ps_.enter_context(nc.sbuf_tensor("rpi", [128, 512], I32)))
            a = Buf(ps_.enter_context(nc.sbuf_tensor("rpa", [128, 512], F32)))
            b_ = Buf(ps_.enter_context(nc.sbuf_tensor("rpb", [128, 512], F32)))
            ki = Buf(ps_.enter_context(nc.sbuf_tensor("rpk", [128, 512], I32)))
            t1 = Buf(ps_.enter_context(nc.sbuf_tensor("rpt", [128, 512], F32)))
            for g in range(NG):
                k.dma("sp", pi_[:, :], pos_in[0:1, g * 512:(g + 1) * 512].partition_broadcast(128), [pos_in], [pi_])
                k.cp(a[:, :], pi_[:, :], [pi_], [a])
                k.ts(a[:, :], a[:, :], ropef[:, 0:1], None, ALU.mult, None, [a, ropef], [a])
                for which, dst in ((0, sind), (1, cosd)):
                    k.ts(b_[:, :], a[:, :], float(np.pi / 2) if which else 0.0, 1.0 / (2 * np.pi), ALU.add, ALU.mult, [a], [b_])
                    k.cp(ki[:, :], b_[:, :], [b_], [ki])
                    k.cp(b_[:, :], ki[:, :], [ki], [b_])
                    k.stt(b_[:, :], b_[:, :], float(-2 * np.pi), a[:, :], ALU.mult, ALU.add, [a, b_], [b_])
                    if which:
                        k.ts(b_[:, :], b_[:, :], float(np.pi / 2), None, ALU.add, None, [b_], [b_])
                    k.ts(t1[:, :], b_[:, :], float(np.pi), float(-2 * np.pi), ALU.is_gt, ALU.mult, [b_], [t1])
                    k.tt(b_[:, :], b_[:, :], t1[:, :], ALU.add, [b_, t1], [b_])
                    k.ts(t1[:, :], b_[:, :], float(-np.pi), float(2 * np.pi), ALU.is_lt, ALU.mult, [b_], [t1])
                    k.tt(b_[:, :], b_[:, :], t1[:, :], ALU.add, [b_, t1], [b_])
                    k.act(t1[:, :], b_[:, :], AF.Sin, [b_], [t1])
                    if which == 0:
                        k.ts(t1[:, :], t1[:, :], ropef[:, 1:2], None, ALU.mult, None, [t1, ropef], [t1])
                    k.dma("sp", dst[:, g * 512:(g + 1) * 512], t1[:, :], [t1], [dst])
            k.barrier()

        def gelu(out, z, t1, t2, R, W, Tt1, Tt2):
            k.act(t1, z, AF.Square, R, [Tt1])
            k.ts(t1, t1, 0.044715, 1.0, ALU.mult, ALU.add, [Tt1], [Tt1])
            k.tt(t1, t1, z, ALU.mult, [Tt1] + R, [Tt1])
            k.act(t2, t1, AF.Sigmoid, [Tt1], [Tt2], scale=1.5957691216057308)
            k.tt(out, t2, z, ALU.mult, [Tt2] + R, W)

        def ln_featmajor(xs, n, onesM, scr, bank_m, bank_e, out_fn):
            sq, mean, rstd = scr["sq"], scr["mean"], scr["rstd"]
            nchunk = len(xs)
            for i, (xa, xb) in enumerate(xs):
                k.mm(pb(bank_m, n), onesM[:, :], xa, i == 0, i == nchunk - 1, [onesM, xb], [PB[bank_m]])
            for i, (xa, xb) in enumerate(xs):
                k.act(sq[:, 0:n], xa, AF.Square, [xb], [sq])
                k.mm(pb(bank_e, n), onesM[:, :], sq[:, 0:n], i == 0, i == nchunk - 1, [onesM, sq], [PB[bank_e]])
            k.cp(mean[:, 0:n], pb(bank_m, n), [PB[bank_m]], [mean], eng="act")
            k.tt(rstd[:, 0:n], mean[:, 0:n], mean[:, 0:n], ALU.mult, [mean], [rstd])
            k.tt(rstd[:, 0:n], pb(bank_e, n), rstd[:, 0:n], ALU.subtract, [PB[bank_e], rstd], [rstd])
            k.act(rstd[:, 0:n], rstd[:, 0:n], AF.Sqrt, [rstd], [rstd], bias=EPS)
            k.op("dve", lambda: nc.vector.reciprocal(rstd[:, 0:n], rstd[:, 0:n]), [rstd], [rstd])
            for i, (xa, xb) in enumerate(xs):
                out_fn(i, xa, xb, mean[:, 0:n], rstd[:, 0:n], mean, rstd)

        for l in range(nlayers):
            last = (l == nlayers - 1)
            with contextlib.ExitStack() as pa:
                def psb(name, shape, dt=F32):
                    return Buf(pa.enter_context(nc.sbuf_tensor(name + "_%d" % l, list(shape), dt)))
                NW = 3
                wt = [psb("wt%d" % i, [128, 8, 128], BF16) for i in range(NW)]
                wt2 = [psb("wt2_%d" % i, [128, 8, 128], BF16) for i in range(NW)]
                br = [psb("br%d" % i, [1, 256], BF16) for i in range(NW)]
                cosb = [psb("cosb%d" % i, [128, 512]) for i in range(2)]
                sinb = [psb("sinb%d" % i, [128, 512]) for i in range(2)]
                stg = [psb("stg%d" % i, [128, 512], BF16) for i in range(3)]
                stgf = [psb("stgf%d" % i, [128, 512]) for i in range(3)]
                t1s = [psb("t1s%d" % i, [128, 512]) for i in range(2)]
                t2s = [psb("t2s%d" % i, [128, 512]) for i in range(2)]
                wi = w_in.h[l].rearrange("(c p) n -> p c n", p=128)
                wsw = w_sw.h[l].rearrange("(c p) n -> p c n", p=128)
                cnt = {"w": 0, "pb": 0, "s": 0, "t": 0}

                def load_w(dst, dstb, boff, srcw, srcb, srcB, srcbB, c0, n, roff=0):
                    k.dma("pool", dst[:, :, roff:roff + n], srcw[:, :, c0:c0 + n], [srcB], [dst])
                    k.dma("pool", dstb[0:1, boff + roff:boff + roff + n], srcb[0:1, c0:c0 + n], [srcbB], [dstb])

                def proj(bank, w, bt, boff, nrows, g):
                    for c in range(8):
                        k.mm(pb(bank, 512, 0, nrows), w[:, c, 0:nrows], xT_bf[:, c, g * 512:(g + 1) * 512], c == 0, False,
                             [w, xT_bf], [PB[bank]])
                    k.mm(pb(bank, 512, 0, nrows), bt[0:1, boff:boff + nrows], ones_b[0:1, 0:512], False, True,
                         [bt, ones_b], [PB[bank]])

                def rope_tile(c0, s0, nrows, writer):
                    i = cnt["w"] % NW
                    cnt["w"] += 1
                    load_w(wt[i], br[i], 0, wi, b_in.h[l], w_in, b_in, c0, nrows)
                    load_w(wt2[i], br[i], 128, wsw, b_sw.h[l], w_sw, b_sw, s0, nrows)
                    for g in range(NG):
                        ci = g % 2
                        k.dma("sp", cosb[ci][0:nrows, :], cosd[0:nrows, g * 512:(g + 1) * 512], [cosd], [cosb[ci]])
                        k.dma("sp", sinb[ci][0:nrows, :], sind[0:nrows, g * 512:(g + 1) * 512], [sind], [sinb[ci]])
                        b0 = cnt["pb"] % 2 * 2
                        cnt["pb"] += 1
                        proj(b0, wt[i], br[i], 0, nrows, g)
                        proj(b0 + 1, wt2[i], br[i], 128, nrows, g)
                        ti = cnt["t"] % 2
                        cnt["t"] += 1
                        si = cnt["s"] % 3
                        cnt["s"] += 1
                        k.tt(t1s[ti][0:nrows, :], pb(b0, 512, 0, nrows), cosb[ci][0:nrows, :], ALU.mult, [PB[b0], cosb[ci]], [t1s[ti]])
                        k.tt(t2s[ti][0:nrows, :], pb(b0 + 1, 512, 0, nrows), sinb[ci][0:nrows, :], ALU.mult, [PB[b0 + 1], sinb[ci]], [t2s[ti]])
                        k.tt(stg[si][0:nrows, :], t1s[ti][0:nrows, :], t2s[ti][0:nrows, :], ALU.add, [t1s[ti], t2s[ti]], [stg[si]])
                        writer(stg[si], g)

                def wr_q(hh):
                    def f(st_, g):
                        k.dma("sp", qn[2 * hh, :, g * 512:(g + 1) * 512], st_[0:64, :], [st_], [qn])
                        k.dma("sp", qn[2 * hh + 1, :, g * 512:(g + 1) * 512], st_[64:128, :], [st_], [qn])
                    return f
                rope_tile(O_NQ, 0, 128, wr_q(0))
                rope_tile(O_NQ + 128, 128, 128, wr_q(1))

                def wr_k(d0, d1):
                    def f(st_, g):
                        k.dma("sp", d0[:, g * 512:(g + 1) * 512], st_[0:64, :], [st_], [d0])
                        if d1 is not None:
                            k.dma("sp", d1[:, g * 512:(g + 1) * 512], st_[64:128, :], [st_], [d1])
                    return f
                rope_tile(O_NKC, 256, 64, wr_k(kcT, None))
                rope_tile(O_NKS, 320, 64, wr_k(ksT, None))
                rope_tile(O_NKW, 384, 64, wr_k(kwT, None))

                def plain_tile(c0, nrows, post):
                    i = cnt["w"] % NW
                    cnt["w"] += 1
                    load_w(wt[i], br[i], 0, wi, b_in.h[l], w_in, b_in, c0, nrows)
                    for g in range(NG):
                        b0 = cnt["pb"] % 4
                        cnt["pb"] += 1
                        proj(b0, wt[i], br[i], 0, nrows, g)
                        post(b0, g)

                def post_copy(dsts, scale):
                    def f(b0, g):
                        si = cnt["s"] % 3
                        cnt["s"] += 1
                        nr = 64 * len(dsts)
                        k.act(stg[si][0:nr, :], pb(b0, 512, 0, nr), AF.Copy, [PB[b0]], [stg[si]], scale=scale)
                        for j, (dd, sl) in enumerate(dsts):
                            k.dma("sp", sl(g), stg[si][64 * j:64 * j + 64, :], [stg[si]], [dd])
                    return f
                plain_tile(O_NVC, 64, post_copy([(vcT, lambda g: vcT[:, g * 512:(g + 1) * 512])], 1.0))
                for hh in range(2):
                    plain_tile(O_FQ + 128 * hh, 128, post_copy(
                        [(fqa, (lambda h_: (lambda g: fqa[h_, 0:64, g * 512:(g + 1) * 512]))(2 * hh)),
                         (fqa, (lambda h_: (lambda g: fqa[h_, 0:64, g * 512:(g + 1) * 512]))(2 * hh + 1))], 0.125))
                    plain_tile(O_FK + 128 * hh, 128, post_copy(
                        [(fka, (lambda h_: (lambda g: fka[h_, 0:64, g * 512:(g + 1) * 512]))(2 * hh)),
                         (fka, (lambda h_: (lambda g: fka[h_, 0:64, g * 512:(g + 1) * 512]))(2 * hh + 1))], 1.0))

                def post_gelu(ct):
                    def f(b0, g):
                        si = cnt["s"] % 3
                        cnt["s"] += 1
                        ti = cnt["t"] % 2
                        cnt["t"] += 1
                        gelu(stg[si][:, :], pb(b0), t1s[ti][:, :], t2s[ti][:, :], [PB[b0]], [stg[si]], t1s[ti], t2s[ti])
                        k.dma("sp", guT[ct * 128:(ct + 1) * 128, g * 512:(g + 1) * 512], stg[si][:, :], [stg[si]], [guT])
                    return f
                for ct in range(2):
                    plain_tile(O_GU + 128 * ct, 128, post_gelu(ct))

                for ct in range(2):
                    i = cnt["w"] % NW
                    cnt["w"] += 1
                    load_w(wt[i], br[i], 0, wi, b_in.h[l], w_in, b_in, O_CA + 128 * ct, 128)
                    load_w(wt2[i], br[i], 128, wi, b_in.h[l], w_in, b_in, O_CB + 128 * ct, 128)
                    for g in range(NG):
                        b0 = cnt["pb"] % 2 * 2
                        cnt["pb"] += 1
                        proj(b0, wt[i], br[i], 0, 128, g)
                        proj(b0 + 1, wt2[i], br[i], 128, 128, g)
                        ti = cnt["t"] % 2
                        cnt["t"] += 1
                        si = cnt["s"] % 3
                        cnt["s"] += 1
                        k.act(t1s[ti][:, :], pb(b0 + 1), AF.Sigmoid, [PB[b0 + 1]], [t1s[ti]])
                        k.tt(stg[si][:, :], pb(b0), t1s[ti][:, :], ALU.mult, [PB[b0], t1s[ti]], [stg[si]])
                        k.dma("sp", hcT[ct * 128:(ct + 1) * 128, 32 + g * 512:32 + (g + 1) * 512], stg[si][:, :], [stg[si]], [hcT])

                i = cnt["w"] % NW
                cnt["w"] += 1
                load_w(wt[i], br[i], 0, wi, b_in.h[l], w_in, b_in, O_FF, 4)
                cum = [psb("cum%d" % j, [4, 512]) for j in range(2)]
                ffe = psb("ffe", [4, 512])
                aug = [psb("aug%d" % j, [4, 12, 512], BF16) for j in range(2)]
                r1 = psb("ffr1", [4, 512])
                r2 = psb("ffr2", [4, 512])
                for j in range(2):
                    k.memset(aug[j][:, 3:9, :], 1.0, [aug[j]])
                for g in range(NG):
                    b0 = cnt["pb"] % 4
                    cnt["pb"] += 1
                    proj(b0, wt[i], br[i], 0, 4, g)
                    k.act(ffe[:, :], pb(b0, 512, 0, 4), AF.Exp, [PB[b0]], [ffe], scale=-1.0)
                    k.act(ffe[:, :], ffe[:, :], AF.Ln, [ffe], [ffe], bias=1.0)
                    cj = cum[g % 2]
                    cp_ = cum[(g + 1) % 2]
                    init = 0.0 if g == 0 else cp_[:, 511:512]
                    k.op("dve", lambda: nc.vector.tensor_tensor_scan(out=cj[:, :], data0=ones_f[0:4, 0:512], data1=ffe[:, :],
                                                                      initial=init, op0=ALU.mult, op1=ALU.subtract),
                         [ones_f, ffe, cp_], [cj])
                    ag = aug[g % 2]
                    k.cp(ag[:, 0, :], cj[:, :], [cj], [ag], multi=True)
                    k.tt(r1[:, :], cj[:, :], ag[:, 0, :], ALU.subtract, [cj, ag], [r1])
                    k.cp(ag[:, 1, :], r1[:, :], [r1], [ag], multi=True)
                    k.tt(r2[:, :], r1[:, :], ag[:, 1, :], ALU.subtract, [r1, ag], [r2])
                    k.cp(ag[:, 2, :], r2[:, :], [r2], [ag], multi=True)
                    k.ts(ag[:, 9:12, :], ag[:, 0:3, :], -1.0, None, ALU.mult, None, [ag], [ag], multi=True)
                    for h_ in range(4):
                        k.dma("sp", fqa[h_, 64:70, g * 512:(g + 1) * 512], ag[h_:h_ + 1, 0:6, :], [ag], [fqa])
                        k.dma("sp", fka[h_, 64:70, g * 512:(g + 1) * 512], ag[h_:h_ + 1, 6:12, :], [ag], [fka])

                wtm = psb("wtm", [128, 8, 652], BF16)
                btm = psb("btm", [1, 652], BF16)
                offs = 0
                for (c0, n) in ((O_NVS, 64), (O_NVW, 64), (O_FV, 256), (O_NG, 12), (O_GV, 256)):
                    k.dma("pool", wtm[:, :, offs:offs + n], wi[:, :, c0:c0 + n], [w_in], [wtm])
                    k.dma("pool", btm[0:1, offs:offs + n], b_in.h[l][0:1, c0:c0 + n], [b_in], [btm])
                    offs += n
                lng = psb("lng", [128, 256])
                lnb = psb("lnb", [128, 256])
                k.dma("sp", lng[:, :], g_lng.h[l][0:1, :].partition_broadcast(128), [g_lng], [lng])
                k.dma("sp", lnb[:, :], g_lnb.h[l][0:1, :].partition_broadcast(128), [g_lnb], [lnb])
                vst = [psb("vst%d" % j, [128, 6, 65], BF16) for j in range(2)]
                gvb = [psb("gvb%d" % j, [128, 256], BF16) for j in range(2)]
                gv1 = psb("gv1", [128, 256])
                gv2 = psb("gv2", [128, 256])
                gv3 = psb("gv3", [128, 256])
                bst = psb("bst", [128, 6])
                mv = psb("mv", [128, 2])
                for j in range(2):
                    k.memset(vst[j][:, :, 64:65], 1.0, [vst[j]])
                for tt_ in range(NT):
                    bA = 4 + (tt_ % 2) * 2
                    bB = bA + 1
                    for (bk, o0, n) in ((bA, 0, 384), (bB, 384, 268)):
                        for c in range(8):
                            k.mm(pb(bk, n), xT_bf[:, c, tt_ * 128:(tt_ + 1) * 128], wtm[:, c, o0:o0 + n], c == 0, False,
                                 [xT_bf, wtm], [PB[bk]])
                        k.mm(pb(bk, n), ones_b[0:1, 0:128], btm[0:1, o0:o0 + n], False, True, [ones_b, btm], [PB[bk]])
                    vs_ = vst[tt_ % 2]
                    k.cp(vs_[:, :, 0:64], psum[:, bA, 0:384].rearrange("p (a b) -> p a b", b=64), [PB[bA]], [vs_], eng="act", multi=True)
                    k.dma("sp", vaug[tt_ * 128:(tt_ + 1) * 128, :, :], vs_[:, :, :], [vs_], [vaug])
                    k.act(gates_r[:, tt_, :], pb(bB, 12), AF.Sigmoid, [PB[bB]], [gates_r], multi=True)
                    z = psum[:, bB, 12:268]
                    gelu(gv3[:, :], z, gv1[:, :], gv2[:, :], [PB[bB]], [gv3], gv1, gv2)
                    k.op("dve", lambda: nc.vector.bn_stats(out=bst[:, :], in_=gv3[:, :]), [gv3], [bst])
                    k.op("dve", lambda: nc.vector.bn_aggr(out=mv[:, :], in_=bst[:, :]), [bst], [mv])
                    k.act(mv[:, 1:2], mv[:, 1:2], AF.Sqrt, [mv], [mv], bias=EPS)
                    k.op("dve", lambda: nc.vector.reciprocal(mv[:, 1:2], mv[:, 1:2]), [mv], [mv])
                    k.ts(gv1[:, :], gv3[:, :], mv[:, 0:1], mv[:, 1:2], ALU.subtract, ALU.mult, [gv3, mv], [gv1])
                    k.tt(gv1[:, :], gv1[:, :], lng[:, :], ALU.mult, [gv1, lng], [gv1])
                    gb = gvb[tt_ % 2]
                    k.tt(gb[:, :], gv1[:, :], lnb[:, :], ALU.add, [gv1, lnb], [gb])
                    k.dma("sp", gvn[tt_ * 128:(tt_ + 1) * 128, :], gb[:, :], [gb], [gvn])
            k.barrier()
            if stop == "A":
                break

            with contextlib.ExitStack() as pbk:
                def psb(name, shape, dt=F32):
                    return Buf(pbk.enter_context(nc.sbuf_tensor(name + "_%d" % l, list(shape), dt)))
                kcmpT = psb("kcmpT", [64, 256], BF16)
                vcaug = psb("vcaug", [128, 2, 129], BF16)
                k.dma("pool", vcaug[:, :, 64:129], cin["ovl"][:, :, :], [cin["ovl"]], [vcaug])
                with contextlib.ExitStack() as pc:
                    def csb(name, shape, dt=F32):
                        return Buf(pc.enter_context(nc.sbuf_tensor(name + "_%d" % l, list(shape), dt)))
                    kin = csb("kin", [64, S + 16], BF16)
                    k.memset(kin[:, S:S + 16], 0.0, [kin], multi=True)
                    w1b = csb("w1b", [64, 32, 64], BF16)
                    w2b = csb("w2b", [64, 64], BF16)
                    peT = csb("peT", [64, 32], BF16)
                    peB = csb("peB", [64, 32, 256], BF16)
                    g1 = csb("g1", [64, 256], BF16)
                    ct1 = csb("ct1", [64, 256])
                    ct2 = csb("ct2", [64, 256])
                    for which in range(2):
                        src, w1s, w2s, pes = ((kcT, cw1k, cw2k, pe_kT), (vcT, cw1v, cw2v, pe_vT))[which]
                        k.dma("sp", kin[:, 0:S], src[:, :], [src], [kin])
                        k.dma("pool", w1b[:, :, :], w1s.h[l].rearrange("(j d) h -> d j h", d=64), [w1s], [w1b])
                        k.dma("pool", w2b[:, :], w2s.h[l], [w2s], [w2b])
                        k.dma("pool", peT[:, :], pes.h[l], [pes], [peT])
                        k.cp(peB[:, :, :], peT[:, :].unsqueeze(2).to_broadcast([64, 32, 256]), [peT], [peB])
                        for j in range(32):
                            k.mm(pb(0, 256, 0, 64), w1b[:, j, :], kin[:, j:j + 16 * 255 + 1:16], j == 0, False, [w1b, kin], [PB[0]])
                        for j in range(32):
                            k.mm(pb(0, 256, 0, 64), w1b[:, j, :], peB[:, j, :], False, j == 31, [w1b, peB], [PB[0]])
                        gelu(g1[:, :], pb(0, 256, 0, 64), ct1[:, :], ct2[:, :], [PB[0]], [g1], ct1, ct2)
                        if which == 0:
                            k.mm(pb(1, 256, 0, 64), w2b[:, :], g1[:, :], True, True, [w2b, g1], [PB[1]])
                            k.cp(kcmpT[:, :], pb(1, 256, 0, 64), [PB[1]], [kcmpT])
                        else:
                            for ct in range(2):
                                k.mm(pb(2 + ct, 64), g1[:, ct * 128:(ct + 1) * 128], w2b[:, :], True, True, [g1, w2b], [PB[2 + ct]])
                                k.cp(vcaug[:, ct, 0:64], pb(2 + ct, 64), [PB[2 + ct]], [vcaug], multi=True)
                    k.barrier()
                if stop == "B1":
                    break

                qh = psb("qh", [64, 4, S], BF16)
                ksb = psb("ksb", [64, S], BF16)
                kwb = psb("kwb", [64, S], BF16)
                vsw = psb("vsw", [128, NT, 2, 65], BF16)
                ebig = psb("ebig", [64, S], BF16)
                k.dma("sp", qh[:, :, :], qn.h.rearrange("h d s -> d h s"), [qn], [qh])
                k.dma("sp", ksb[:, :], ksT[:, :], [ksT], [ksb])
                k.dma("sp", kwb[:, :], kwT[:, :], [kwT], [kwb])
                for t_ in range(NT):
                    k.dma("sp", vsw[:, t_, :, :], vaug[t_ * 128:(t_ + 1) * 128, 0:2, :], [vaug], [vsw])
                k.dma("pool", ebig[:, :], cin["ebig"][:, :], [cin["ebig"]], [ebig], max_dma_last_dim=4096)
                cmk = [psb("cmk%d" % j, [128, 2, 128], BF16) for j in range(2)]
                stb = [psb("stb%d" % j, [128, 2, 64]) for j in range(2)]
                NPT = 6
                pT = [psb("pT%d" % j, [128, 4, 128], BF16) for j in range(NPT)]
                imp = psb("imp", [128, 64])
                imp2 = psb("imp2", [128, 64])
                mx = psb("mx", [128, 8])
                selm = psb("selm", [128, 64])
                selT = psb("selT", [64, 128], BF16)
                den = psb("den", [128, 12])
                coef = psb("coef", [128, 12])
                acc_c = psb("acc_c", [128, 4, 64])
                onsa = psb("onsa", [128, 256])
                onT = [psb("onT%d" % j, [128, 2, 128], BF16) for j in range(2)]
                mcb = psb("mcb", [128, 128], BF16)
                mbb = psb("mbb", [128, 128], BF16)
                k.cp(mcb[:, :], m_causal[:, :], [m_causal], [mcb])
                k.cp(mbb[:, :], m_band[:, :], [m_band], [mbb])
                SBK = [2, 3, 6]
                pc_ = {"s": 0, "p": 0}
                LAG = 2

                def pipe(items, hooks=()):
                    n = len(items)
                    hk = {}
                    for (at, fn) in hooks:
                        hk.setdefault(min(at, n + LAG - 1), []).append(fn)
                    for s_ in range(n + LAG):
                        for fn in hk.get(s_, []):
                            fn()
                        if s_ < n:
                            items[s_][0]()
                        if s_ >= LAG:
                            items[s_ - LAG][1]()

                def bc4(ap, np_=128):
                    return ap.unsqueeze(1).to_broadcast([np_, 4, 128])

                for i in range(NT):
                    q0 = i * 128
                    cm_ = cmk[i % 2]
                    st_ = stb[i % 2]
                    k.dma("pool", cm_[:, :, :], cin["cmask"][i, :, :, :], [cin["cmask"]], [cm_])
                    k.dma("sp", st_[:, :, :], cin["seltab"][i, :, :, :], [cin["seltab"]], [st_])
                    nct = 2 if i >= 16 else 1
                    qv = qh[:, :, q0:q0 + 128]
                    w0 = max(0, i - 4)

                    def mk_item(kind, kt, i=i, cm_=cm_, qv=qv, w0=w0):
                        st8 = {}

                        def front():
                            sbk = SBK[pc_["s"] % 3]
                            pc_["s"] += 1
                            p_ = pT[pc_["p"] % NPT]
                            pc_["p"] += 1
                            st8["p"] = p_
                            if kind == "cmp":
                                k.mm(pb(sbk), kcmpT[:, kt * 128:(kt + 1) * 128], qv, True, False, [kcmpT, qh], [PB[sbk]])
                                k.mm(pb(sbk), ident_b[:, :], bc4(cm_[:, kt, :]), False, True, [ident_b, cm_], [PB[sbk]])
                            elif kind == "sel":
                                last_ = (kt == i)
                                k.mm(pb(sbk), ksb[:, kt * 128:(kt + 1) * 128], qv, True, False, [ksb, qh], [PB[sbk]])
                                k.mm(pb(sbk), ebig[:, kt * 128:(kt + 1) * 128], bc4(selT[:, :], 64), False, not last_, [ebig, selT], [PB[sbk]])
                                if last_:
                                    k.mm(pb(sbk), ident_b[:, :], bc4(mcb[:, :]), False, True, [ident_b, mcb], [PB[sbk]])
                            else:
                                masked = (kt == i or kt == i - 4)
                                k.mm(pb(sbk), kwb[:, kt * 128:(kt + 1) * 128], qv, True, not masked, [kwb, qh], [PB[sbk]])
                                if masked:
                                    mk = mcb if kt == i else mbb
                                    k.mm(pb(sbk), ident_b[:, :], bc4(mk[:, :]), False, True, [ident_b, mk], [PB[sbk]])
                            k.act(p_[:, :, :], psum[:, sbk, :].rearrange("p (h q) -> p h q", h=4), AF.Exp, [PB[sbk]], [p_], scale=0.125)

                        def back():
                            p_ = st8["p"]
                            if kind == "cmp":
                                for h_ in range(4):
                                    bk = h_ // 2
                                    k.mm(psum[:, bk, (h_ % 2) * 129:(h_ % 2) * 129 + 129], p_[:, h_, :], vcaug[:, kt, :],
                                         (kt == 0 and h_ % 2 == 0), False, [p_, vcaug], [PB[bk]], skip_group_check=True)
                            else:
                                bk, vi, first = (4, 0, kt == 0) if kind == "sel" else (5, 1, kt == w0)
                                if first:
                                    k.mm(pb(bk, 260), zeros_b[0:1, 0:128], zeros_b[0:1, 0:260], True, False, [zeros_b], [PB[bk]], skip_group_check=True)
                                for h_ in range(4):
                                    k.mm(psum[:, bk, h_ * 65:h_ * 65 + 65], p_[:, h_, :], vsw[:, kt, vi, :], False, False, [p_, vsw], [PB[bk]],
                                         skip_group_check=True)
                        return (front, back)

                    pipe([mk_item("cmp", ct) for ct in range(nct)])
                    for h_ in range(4):
                        bk = h_ // 2
                        o_ = (h_ % 2) * 129
                        k.ts(den[:, h_:h_ + 1], psum[:, bk, o_ + 64:o_ + 65], 1e-30, None, ALU.max, None, [PB[bk]], [den], multi=True)
                    k.op("dve", lambda: nc.vector.reciprocal(coef[:, 0:4], den[:, 0:4]), [den], [coef], multi=True)
                    for h_ in range(4):
                        bk = h_ // 2
                        o_ = (h_ % 2) * 129
                        k.cp(acc_c[:, h_, :], psum[:, bk, o_:o_ + 64], [PB[bk]], [acc_c], eng="act", multi=True)
                        if h_ == 0:
                            k.ts(imp[:, :], psum[:, bk, o_ + 65:o_ + 129], coef[:, 0:1], None, ALU.mult, None, [PB[bk], coef], [imp])
                        else:
                            k.stt(imp[:, :], psum[:, bk, o_ + 65:o_ + 129], coef[:, h_:h_ + 1], imp[:, :], ALU.mult, ALU.add,
                                  [PB[bk], coef, imp], [imp])

                    def hookA(st_=st_):
                        k.tt(imp[:, :], imp[:, :], st_[:, 0, :], ALU.mult, [imp, st_], [imp])
                        k.tt(imp[:, :], imp[:, :], st_[:, 1, :], ALU.add, [imp, st_], [imp])
                        k.op("dve", lambda: nc.vector.max(out=mx[:, :], in_=imp[:, :]), [imp], [mx])
                        k.op("dve", lambda: nc.vector.match_replace(out=imp2[:, :], in_to_replace=mx[:, :], in_values=imp[:, :],
                                                                     imm_value=-2.0), [mx, imp], [imp2])
                        k.op("dve", lambda: nc.vector.max(out=mx[:, :], in_=imp2[:, :]), [imp2], [mx])
                        k.ts(selm[:, :], imp[:, :], mx[:, 7:8], None, ALU.is_ge, None, [imp, mx], [selm])
                        k.tt(selm[:, :], selm[:, :], st_[:, 0, :], ALU.mult, [selm, st_], [selm])
                        k.ts(selm[:, :], selm[:, :], -1.0, None, ALU.add, None, [selm], [selm])

                    def hookB():
                        k.tr(pb(7, 128, 0, 64), selm[:, :], ident_f[:, :], [selm, ident_f], [PB[7]])
                        k.cp(selT[:, :], pb(7, 128, 0, 64), [PB[7]], [selT])
                    wins = [mk_item("win", kt) for kt in range(w0, i + 1)]
                    sels = [mk_item("sel", kt) for kt in range(i + 1)]
                    pipe(wins + sels, hooks=((min(1, len(wins)), hookA), (min(3, len(wins)), hookB)))
                    for bi, bk in ((1, 4), (2, 5)):
                        k.ts(den[:, bi * 4:bi * 4 + 4], psum[:, bk, 0:260].rearrange("p (h e) -> p h e", e=65)[:, :, 64], 1e-30, None,
                             ALU.max, None, [PB[bk]], [den], multi=True)
                    k.op("dve", lambda: nc.vector.reciprocal(coef[:, 4:12], den[:, 4:12]), [den], [coef], multi=True)
                    gv_ = gates_r[:, i, :].rearrange("p (h b) -> p b h", b=3)
                    k.tt(coef[:, :].rearrange("p (b h) -> p b h", h=4), coef[:, :].rearrange("p (b h) -> p b h", h=4), gv_, ALU.mult,
                         [coef, gates_r], [coef])
                    for h_ in range(4):
                        oh = onsa[:, h_ * 64:(h_ + 1) * 64]
                        k.ts(oh, acc_c[:, h_, :], coef[:, h_:h_ + 1], None, ALU.mult, None, [acc_c, coef], [onsa], multi=True)
                        k.stt(oh, psum[:, 4, h_ * 65:h_ * 65 + 64], coef[:, 4 + h_:5 + h_], oh, ALU.mult, ALU.add, [PB[4], coef, onsa], [onsa], multi=True)
                        k.stt(oh, psum[:, 5, h_ * 65:h_ * 65 + 64], coef[:, 8 + h_:9 + h_], oh, ALU.mult, ALU.add, [PB[5], coef, onsa], [onsa], multi=True)
                    ot = onT[i % 2]
                    for cc in range(2):
                        k.tr(psum[:, 7, cc * 128:(cc + 1) * 128], onsa[:, cc * 128:(cc + 1) * 128], ident_f[:, :], [onsa, ident_f], [PB[7]])
                    k.cp(ot[:, :, :], psum[:, 7, 0:256].rearrange("p (c q) -> p c q", c=2), [PB[7]], [ot], eng="act")
                    k.dma("sp", oT[0].rearrange("(c p) s -> p c s", p=128)[:, :, q0:q0 + 128], ot[:, :, :], [ot], [oT])
            k.barrier()
            if stop == "B2":
                break

            with contextlib.ExitStack() as pf:
                def psb(name, shape, dt=F32):
                    return Buf(pf.enter_context(nc.sbuf_tensor(name + "_%d" % l, list(shape), dt)))
                qa = [psb("qa%d" % j, [70, S], BF16) for j in range(2)]
                ka = [psb("ka%d" % j, [70, S], BF16) for j in range(2)]
                vf = psb("vf", [128, NT, 4, 65], BF16)
                for t_ in range(NT):
                    k.dma("sp", vf[:, t_, :, :], vaug[t_ * 128:(t_ + 1) * 128, 2:6, :], [vaug], [vf])
                NPF = 6
                pF = [psb("pF%d" % j, [128, 512], BF16) for j in range(NPF)]
                otok = psb("otok", [128, NT, 256])
                rden = psb("rden", [128, 4])
                ofT = [psb("ofT%d" % j, [128, 2, 128], BF16) for j in range(2)]
                mcb = psb("mcbf", [128, 128], BF16)
                k.cp(mcb[:, :], m_causal[:, :], [m_causal], [mcb])
                fc_ = {"n": 0}
                LAG = 2
                for h_ in range(4):
                    q_, k_ = qa[h_ % 2], ka[h_ % 2]
                    k.dma("sp", q_[:, :], fqa[h_, :, :], [fqa], [q_])
                    k.dma("sp", k_[:, :], fka[h_, :, :], [fka], [k_])
                    items = []
                    for g in range(NG):
                        nk = 4 * (g + 1)
                        for kt in range(nk):
                            def mk(g=g, kt=kt, nk=nk, q_=q_, k_=k_, h_=h_):
                                ab = 4 + (g % 2)
                                j = kt - 4 * g
                                c0 = max(j, 0) * 128
                                st8 = {}

                                def front():
                                    sbk = fc_["n"] % 4
                                    p_ = pF[fc_["n"] % NPF]
                                    fc_["n"] += 1
                                    st8["p"] = p_
                                    k.mm(psum[:, sbk, c0:512], k_[:, kt * 128:(kt + 1) * 128], q_[:, g * 512 + c0:(g + 1) * 512], True, j < 0,
                                         [k_, q_], [PB[sbk]])
                                    if j >= 0:
                                        k.mm(psum[:, sbk, c0:c0 + 128], ident_b[:, :], mcb[:, :], False, True, [ident_b, mcb], [PB[sbk]])
                                    k.act(p_[:, c0:512], psum[:, sbk, c0:512], AF.Exp, [PB[sbk]], [p_])

                                def back():
                                    p_ = st8["p"]
                                    if kt == 0:
                                        k.mm(pb(ab, 260), zeros_b[0:1, 0:128], zeros_b[0:1, 0:260], True, False, [zeros_b], [PB[ab]], skip_group_check=True)
                                    for qs in range(max(j, 0), 4):
                                        k.mm(psum[:, ab, qs * 65:qs * 65 + 65], p_[:, qs * 128:(qs + 1) * 128], vf[:, kt, h_, :], False, False,
                                             [p_, vf], [PB[ab]], skip_group_check=True)
                                    if kt == nk - 1:
                                        k.op("dve", lambda: nc.vector.reciprocal(rden[:, :], psum[:, ab, 0:260].rearrange("p (q e) -> p q e", e=65)[:, :, 64]),
                                             [PB[ab]], [rden])
                                        for qs in range(4):
                                            k.ts(otok[:, g * 4 + qs, h_ * 64:(h_ + 1) * 64], psum[:, ab, qs * 65:qs * 65 + 64], rden[:, qs:qs + 1], None,
                                                 ALU.mult, None, [PB[ab], rden], [otok], multi=True)
                                return (front, back)
                            items.append(mk())
                    n_ = len(items)
                    for s_ in range(n_ + LAG):
                        if s_ < n_:
                            items[s_][0]()
                        if s_ >= LAG:
                            items[s_ - LAG][1]()
                for tt_ in range(NT):
                    ot = ofT[tt_ % 2]
                    bk = 6 + tt_ % 2
                    for cc in range(2):
                        k.tr(psum[:, bk, cc * 128:(cc + 1) * 128], otok[:, tt_, cc * 128:(cc + 1) * 128], ident_f[:, :], [otok, ident_f], [PB[bk]])
                    k.cp(ot[:, :, :], psum[:, bk, 0:256].rearrange("p (c q) -> p c q", c=2), [PB[bk]], [ot], eng="act")
                    k.dma("sp", oT[1].rearrange("(c p) s -> p c s", p=128)[:, :, tt_ * 128:(tt_ + 1) * 128], ot[:, :, :], [ot], [oT])
            k.barrier()
            if stop == "B3":
                break

            with contextlib.ExitStack() as pg:
                def psb(name, shape, dt=F32):
                    return Buf(pg.enter_context(nc.sbuf_tensor(name + "_%d" % l, list(shape), dt)))
                wsf = psb("wsf", [128, 4, 128])
                wsb = psb("wsb", [128, 4, 128], BF16)
                tril = psb("tril", [128, 128])
                bsf = psb("bsf", [1, 512])
                bsh = psb("bsh", [1, 512], BF16)
                bsl = psb("bsl", [1, 512], BF16)
                k.dma("sp", wsf[:, :, :], g_wsT.h[l], [g_wsT], [wsf])
                k.dma("sp", tril[:, :], cin["m_tril"][:, :], [cin["m_tril"]], [tril])
                k.dma("sp", bsf[:, :], g_bs.h[l], [g_bs], [bsf])
                k.tt(wsb[:, :, :], wsf[:, :, :], tril[:, :].unsqueeze(1).to_broadcast([128, 4, 128]), ALU.mult, [wsf, tril], [wsb])
                k.cp(bsh[:, :], bsf[:, :], [bsf], [bsh])
                k.tt(bsf[:, :], bsf[:, :], bsh[:, :], ALU.subtract, [bsf, bsh], [bsf])
                k.cp(bsl[:, :], bsf[:, :], [bsf], [bsl])
                gvt = [psb("gvt%d" % j, [128, 256], BF16) for j in range(2)]
                gut = [psb("gut%d" % j, [128, 2, 128], BF16) for j in range(2)]
                ogt = [psb("ogt%d" % j, [128, 2, 128], BF16) for j in range(2)]
                for ch in range(NT):
                    gv_, gu_, og_ = gvt[ch % 2], gut[ch % 2], ogt[ch % 2]
                    k.dma("sp", gv_[:, :], gvn[ch * 128:(ch + 1) * 128, :], [gvn], [gv_])
                    k.dma("sp", gu_[:, :, :], guT.h.rearrange("(c p) s -> p c s", p=128)[:, :, ch * 128:(ch + 1) * 128], [guT], [gu_])
                    bk = ch % 2
                    for gg in range(4):
                        o_ = psum[(gg % 2) * 64:(gg % 2) * 64 + 64, bk, (gg // 2) * 128:(gg // 2) * 128 + 128]
                        k.mm(o_, gv_[:, gg * 64:(gg + 1) * 64], wsb[:, gg, :], True, False, [gv_, wsb], [PB[bk]], skip_group_check=True)
                        k.mm(o_, ones_b[0:1, 0:64], bsh[0:1, gg * 128:(gg + 1) * 128], False, False, [ones_b, bsh], [PB[bk]], skip_group_check=True)
                        k.mm(o_, ones_b[0:1, 0:64], bsl[0:1, gg * 128:(gg + 1) * 128], False, True, [ones_b, bsl], [PB[bk]], skip_group_check=True)
                    k.tt(og_[:, :, :], psum[:, bk, 0:256].rearrange("p (c t) -> p c t", c=2), gu_[:, :, :], ALU.mult, [PB[bk], gu_], [og_])
                    k.dma("sp", oT[2].rearrange("(c p) s -> p c s", p=128)[:, :, ch * 128:(ch + 1) * 128], og_[:, :, :], [og_], [oT])
            k.barrier()
            with contextlib.ExitStack() as pcv:
                def psb(name, shape, dt=F32):
                    return Buf(pcv.enter_context(nc.sbuf_tensor(name + "_%d" % l, list(shape), dt)))
                hin = [psb("hin%d" % j, [128, 32 + S], BF16) for j in range(2)]
                dg = [psb("dg%d" % j, [128, 31, 128], BF16) for j in range(2)]
                cac = [[psb("cac%d_%d" % (j, q_), [128, 512]) for q_ in range(2)] for j in range(2)]
                cwt = [psb("cwt%d" % j, [128, 31]) for j in range(2)]
                cpr = [psb("cpr%d" % j, [128, 3]) for j in range(2)]
                scr = {"sq": psb("c_sq", [128, 512]), "mean": psb("c_mean", [128, 512]), "rstd": psb("c_rstd", [128, 512])}
                cy = [psb("cy%d" % j, [128, 512]) for j in range(2)]
                cyb = [psb("cyb%d" % j, [128, 512], BF16) for j in range(2)]
                for ct in range(2):
                    k.dma("sp", hin[ct][:, :], hcT[ct * 128:(ct + 1) * 128, :], [hcT], [hin[ct]])
                    k.dma("sp", cwt[ct][:, :], c_wT.h[l][ct * 128:(ct + 1) * 128, :], [c_wT], [cwt[ct]])
                    k.dma("sp", cpr[ct][:, :], c_par.h[l][ct * 128:(ct + 1) * 128, :], [c_par], [cpr[ct]])
                    for j in range(31):
                        k.ts(dg[ct][:, j, :], ident_f[:, :], cwt[ct][:, j:j + 1], None, ALU.mult, None, [ident_f, cwt[ct]], [dg[ct]], multi=True)
                cnt5 = [0]
                for g in range(NG):
                    for ct in range(2):
                        bk = 4 + ct + 2 * (g % 2)
                        for j in range(31):
                            k.mm(pb(bk), dg[ct][:, j, :], hin[ct][:, 2 + j + g * 512:2 + j + g * 512 + 512], j == 0, j == 30, [dg[ct], hin[ct]], [PB[bk]])
                        k.act(cac[ct][g % 2][:, :], pb(bk), AF.Identity, [PB[bk], cpr[ct]], [cac[ct][g % 2]], bias=cpr[ct][:, 0:1])
                    xs = [(cac[ct][g % 2][:, :], cac[ct][g % 2]) for ct in range(2)]

                    def outf(i_, xa, xb, mean, rstd, Bm, Br, g=g):
                        y = cy[cnt5[0] % 2]
                        yb = cyb[cnt5[0] % 2]
                        cnt5[0] += 1
                        k.tt(y[:, :], xa, mean, ALU.subtract, [xb, Bm], [y])
                        k.tt(y[:, :], y[:, :], rstd, ALU.mult, [y, Br], [y])
                        k.ts(y[:, :], y[:, :], cpr[i_][:, 1:2], cpr[i_][:, 2:3], ALU.mult, ALU.add, [y, cpr[i_]], [y])
                        k.act(yb[:, :], y[:, :], AF.Silu, [y], [yb])
                        k.dma("sp", oT[3, i_ * 128:(i_ + 1) * 128, g * 512:(g + 1) * 512], yb[:, :], [yb], [oT])
                    ln_featmajor(xs, 512, onesC, scr, 0 + 2 * (g % 2), 1 + 2 * (g % 2), outf)
            k.barrier()
            if stop == "B5":
                break

            with contextlib.ExitStack() as pd:
                def psb(name, shape, dt=F32):
                    return Buf(pd.enter_context(nc.sbuf_tensor(name + "_%d" % l, list(shape), dt)))
                wg = [psb("wg%d" % j, [128, 8, 4, 128], BF16) for j in range(2)]
                bg = [psb("bg%d" % j, [1, 4, 128], BF16) for j in range(2)]
                wb = [psb("wb%d" % j, [128, 4, 2, 128], BF16) for j in range(2)]
                ob = [psb("ob%d" % j, [128, 4, 2, 512], BF16) for j in range(2)]
                sgt = [psb("sgt%d" % j, [128, 512], BF16) for j in range(2)]
                macc = [psb("macc%d" % j, [128, 512]) for j in range(2)]
                mtmp = [psb("mtmp%d" % j, [128, 512]) for j in range(2)]
                mob = [psb("mob%d" % j, [128, 512], BF16) for j in range(2)]
                wi = w_in.h[l].rearrange("(c p) n -> p c n", p=128)
                it = 0
                for dt_ in range(8):
                    w_, b_, wb_ = wg[dt_ % 2], bg[dt_ % 2], wb[dt_ % 2]
                    for n in range(4):
                        c0 = O_MG + n * D + dt_ * 128
                        k.dma("pool", w_[:, :, n, :], wi[:, :, c0:c0 + 128], [w_in], [w_])
                        k.dma("pool", b_[0:1, n, :], b_in.h[l][0:1, c0:c0 + 128], [b_in], [b_])
                        k.dma("pool", wb_[:, n, :, :], w_br.h[l, n].rearrange("(c p) d -> p c d", p=128)[:, :, dt_ * 128:(dt_ + 1) * 128],
                              [w_br], [wb_])
                    for g in range(NG):
                        o_ = ob[it % 2]
                        k.dma("sp", o_[:, :, :, :], oT.h.rearrange("n (c p) s -> p n c s", p=128)[:, :, :, g * 512:(g + 1) * 512], [oT], [o_])
                        ma = macc[it % 2]
                        for n in range(4):
                            bA = (n % 2) * 2 + (it % 2) * 4
                            bB = bA + 1
                            for c in range(8):
                                k.mm(pb(bA), w_[:, c, n, :], xT_bf[:, c, g * 512:(g + 1) * 512], c == 0, False, [w_, xT_bf], [PB[bA]])
                            k.mm(pb(bA), b_[0:1, n, :], ones_b[0:1, 0:512], False, True, [b_, ones_b], [PB[bA]])
                            for c in range(2):
                                k.mm(pb(bB), wb_[:, n, c, :], o_[:, n, c, :], c == 0, c == 1, [wb_, o_], [PB[bB]])
                            sg_ = sgt[n % 2]
                            k.act(sg_[:, :], pb(bA), AF.Sigmoid, [PB[bA]], [sg_])
                            if n == 0:
                                k.tt(ma[:, :], pb(bB), sg_[:, :], ALU.mult, [PB[bB], sg_], [ma])
                            else:
                                mt = mtmp[n % 2]
                                k.tt(mt[:, :], pb(bB), sg_[:, :], ALU.mult, [PB[bB], sg_], [mt])
                                if n < 3:
                                    k.tt(ma[:, :], ma[:, :], mt[:, :], ALU.add, [ma, mt], [ma], eng="pool")
                                else:
                                    mo = mob[it % 2]
                                    k.tt(mo[:, :], ma[:, :], mt[:, :], ALU.add, [ma, mt], [mo], eng="pool")
                                    k.dma("sp", mixT[dt_ * 128:(dt_ + 1) * 128, g * 512:(g + 1) * 512], mo[:, :], [mo], [mixT])
                        it += 1
            k.barrier()
            if stop == "D1":
                break

            with contextlib.ExitStack() as pm:
                def msb(name, shape, dt=F32):
                    return Buf(pm.enter_context(nc.sbuf_tensor(name + "_%d" % l, list(shape), dt)))
                wts_all = msb("wts_all", [128, NT, 4])
                dest_all = msb("dest_all", [128, NT, 4], I32)
                scr = {"sq": msb("l_sq", [128, 512]), "mean": msb("l_mean", [128, 512]), "rstd": msb("l_rstd", [128, 512])}

                def resid_ln(xr, lnp, g, final_dst):
                    xs = [(xr[:, c, :], xr) for c in range(8)]

                    def outf(i_, xa, xb, mean, rstd, Bm, Br):
                        k.tt(xa, xa, mean, ALU.subtract, [xb, Bm], [xb], multi=True)
                        k.tt(xa, xa, rstd, ALU.mult, [xb, Br], [xb], multi=True)
                        k.ts(xa, xa, lnp[:, i_, 0:1], lnp[:, i_, 1:2], ALU.mult, ALU.add, [xb, lnp], [xb], multi=True)
                        k.cp(xT_bf[:, i_, g * 512:(g + 1) * 512], xa, [xb], [xT_bf], eng="act", multi=True)
                        k.dma("sp", final_dst[i_ * 128:(i_ + 1) * 128, g * 512:(g + 1) * 512], xa, [xb], [final_dst])
                    ln_featmajor(xs, 512, onesD, scr, 6, 7, outf)

                with contextlib.ExitStack() as pd2:
                    def psb(name, shape, dt=F32):
                        return Buf(pd2.enter_context(nc.sbuf_tensor(name + "_%d" % l, list(shape), dt)))
                    wo = psb("wo", [128, 8, D], BF16)
                    for c in range(8):
                        k.dma("pool", wo[:, c, :], w_o.h[l][c * 128:(c + 1) * 128, :], [w_o], [wo])
                    ln1p = psb("ln1p", [128, 8, 2])
                    k.dma("sp", ln1p[:, :, :], ln1.h[l], [ln1], [ln1p])
                    wr = psb("wr", [128, 8, NE])
                    brr = psb("brr", [1, NE])
                    k.dma("sp", wr[:, :, :], w_r.h[l].rearrange("(c p) e -> p c e", p=128), [w_r], [wr])
                    k.dma("sp", brr[:, :], b_r.h[l], [b_r], [brr])
                    ustr = psb("ustr", [128, 128], BF16)
                    k.dma("pool", ustr[:, :], cin["u_strict"][:, :], [cin["u_strict"]], [ustr])
                    iota = psb("iota", [128, 32])
                    k.dma("sp", iota[:, :], cin["iota32"][:, :], [cin["iota32"]], [iota])
                    mcum = psb("mcum", [128, NE], BF16)
                    k.memset(mcum[:, :], 0.0, [mcum])
                    mp = [psb("mp%d" % j, [128, 8, 512], BF16) for j in range(2)]
                    xr = [psb("xr%d" % j, [128, 8, 512]) for j in range(2)]
                    lg = psb("lg", [128, NE])
                    mx8 = psb("mx8", [128, 8])
                    ix8 = psb("ix8", [128, 8], U32)
                    ixf = psb("ixf", [128, 4])
                    nmx = psb("nmx", [128, 1])
                    ew = psb("ew", [128, 4])
                    esum = psb("esum", [128, 1])
                    mskf = psb("mskf", [128, NE])
                    mskb = psb("mskb", [128, NE], BF16)
                    oh = psb("oh", [128, NE])
                    ohj = psb("ohj", [128, NE])
                    posk = psb("posk", [128, 4])
                    dstf = psb("dstf", [128, 4])
                    xtok = [psb("xtok%d" % j, [128, D], BF16) for j in range(2)]
                    for g in range(NG):
                        m_, x_ = mp[g % 2], xr[g % 2]
                        k.dma("sp", m_[:, :, :], mixT.h.rearrange("(c p) s -> p c s", p=128)[:, :, g * 512:(g + 1) * 512], [mixT], [m_])
                        k.dma("sp", x_[:, :, :], xres.h.rearrange("(c p) s -> p c s", p=128)[:, :, g * 512:(g + 1) * 512], [xres], [x_])
                        for dd in range(8):
                            bk = dd % 4
                            for c in range(8):
                                k.mm(pb(bk), wo[:, c, dd * 128:(dd + 1) * 128], m_[:, c, :], c == 0, c == 7, [wo, m_], [PB[bk]])
                            k.stt(x_[:, dd, :], x_[:, dd, :], ALPHA, pb(bk), ALU.mult, ALU.add, [x_, PB[bk]], [x_], multi=True)
                        resid_ln(x_, ln1p, g, xres)
                        for t4 in range(4):
                            tt_ = g * 4 + t4
                            for c in range(8):
                                k.mm(pb(4, NE), x_[:, c, t4 * 128:(t4 + 1) * 128], wr[:, c, :], c == 0, False, [x_, wr], [PB[4]])
                            k.mm(pb(4, NE), ones_f[0:1, 0:128], brr[0:1, :], False, True, [ones_f, brr], [PB[4]])
                            k.cp(lg[:, :], pb(4, NE), [PB[4]], [lg])
                            k.op("dve", lambda: nc.vector.max(out=mx8[:, :], in_=lg[:, :]), [lg], [mx8])
                            k.op("dve", lambda: nc.vector.max_index(out=ix8[:, :], in_max=mx8[:, :], in_values=lg[:, :]), [mx8, lg], [ix8])
                            k.ts(nmx[:, :], mx8[:, 0:1], -1.0, None, ALU.mult, None, [mx8], [nmx])
                            k.act(ew[:, :], mx8[:, 0:4], AF.Exp, [mx8, nmx], [ew], bias=nmx[:, 0:1])
                            k.op("dve", lambda: nc.vector.reduce_sum(out=esum[:, :], in_=ew[:, :], axis=mybir.AxisListType.X), [ew], [esum])
                            k.op("dve", lambda: nc.vector.reciprocal(esum[:, :], esum[:, :]), [esum], [esum])
                            k.ts(wts_all[:, tt_, :], ew[:, :], esum[:, 0:1], None, ALU.mult, None, [ew, esum], [wts_all], multi=True)
                            k.ts(mskf[:, :], lg[:, :], mx8[:, 3:4], None, ALU.is_ge, None, [lg, mx8], [mskf])
                            k.cp(mskb[:, :], mskf[:, :], [mskf], [mskb])
                            k.mm(pb(5, NE), ustr[:, :], mskb[:, :], True, False, [ustr, mskb], [PB[5]])
                            k.mm(pb(5, NE), ones_b[:, 0:128], mcum[:, :], False, True, [ones_b, mcum], [PB[5]])
                            k.cp(ixf[:, :], ix8[:, 0:4], [ix8], [ixf])
                            for kk in range(4):
                                k.ts(oh[:, :], iota[:, :], ixf[:, kk:kk + 1], None, ALU.is_equal, None, [iota, ixf], [oh])
                                k.tt(ohj[:, :], oh[:, :], pb(5, NE), ALU.mult, [oh, PB[5]], [ohj])
                                k.op("dve", lambda: nc.vector.reduce_sum(out=posk[:, kk:kk + 1], in_=ohj[:, :], axis=mybir.AxisListType.X),
                                     [ohj], [posk], multi=True)
                            k.tt(mcum[:, :], mcum[:, :], mskb[:, :], ALU.add, [mcum, mskb], [mcum])
                            k.ts(posk[:, :], posk[:, :], float(CAP - 1), None, ALU.min, None, [posk], [posk])
                            k.stt(dstf[:, :], ixf[:, :], float(CAP), posk[:, :], ALU.mult, ALU.add, [ixf, posk], [dstf])
                            k.cp(dest_all[:, tt_, :], dstf[:, :], [dstf], [dest_all], multi=True)
                            xt_ = xtok[tt_ % 2]
                            bk = 2 + tt_ % 2
                            for c in range(8):
                                k.tr(pbb(bk)[:, c * 128:(c + 1) * 128], xT_bf[:, c, tt_ * 128:(tt_ + 1) * 128], ident_b[:, :], [xT_bf, ident_b], [PB[bk]])
                            k.cp(xt_[:, :], pbb(bk)[:, :], [PB[bk]], [xt_], eng="act")
                            for kk in range(4):
                                k.dma("pool", xe_d[:, :], xt_[:, :], [xt_, dest_all], [xe_d],
                                      indirect=dict(out_offset=bass.IndirectOffsetOnAxis(ap=dest_all[:, tt_, kk:kk + 1], axis=0), in_offset=None))
                k.barrier()
                if stop == "D2":
                    break

                with contextlib.ExitStack() as pe_:
                    def psb(name, shape, dt=F32):
                        return Buf(pe_.enter_context(nc.sbuf_tensor(name + "_%d" % l, list(shape), dt)))
                    xel = [psb("xel%d" % j, [128, D], BF16) for j in range(3)]
                    xeT = [psb("xeT%d" % j, [128, 8, CAP], BF16) for j in range(2)]
                    w1s = [psb("w1s%d" % j, [128, 8, 2, 128], BF16) for j in range(3)]
                    b1s = [psb("b1s%d" % j, [1, 2, D], BF16) for j in range(2)]
                    w2s = [psb("w2s%d" % j, [128, 8, D], BF16) for j in range(2)]
                    b2s = [psb("b2s%d" % j, [1, D], BF16) for j in range(2)]
                    aT = [psb("aT%d" % j, [128, 8, CAP], BF16) for j in range(2)]
                    eg = [psb("eg%d" % j, [128, 320]) for j in range(2)]
                    el = [psb("el%d" % j, [128, 320]) for j in range(2)]
                    esg = [psb("esg%d" % j, [128, 320]) for j in range(2)]
                    yst = [psb("yst%d" % j, [128, D], BF16) for j in range(2)]
                    nx = 0
                    nw = 0
                    ne_ = 0
                    HS = CAP // 2
                    for e in range(NE):
                        xT_ = xeT[e % 2]
                        a_ = aT[e % 2]
                        for st in range(CAP // 128):
                            xl = xel[nx % 3]
                            bk = nx % 2
                            nx += 1
                            k.dma("sp", xl[:, :], xe_d[e * CAP + st * 128:e * CAP + (st + 1) * 128, :], [xe_d], [xl])
                            for c in range(8):
                                k.tr(pbb(bk)[:, c * 128:(c + 1) * 128], xl[:, c * 128:(c + 1) * 128], ident_b[:, :], [xl, ident_b], [PB[bk]])
                            k.cp(xT_[:, :, st * 128:(st + 1) * 128], pbb(bk)[:, :].rearrange("p (c s) -> p c s", c=8), [PB[bk]], [xT_],
                                 eng="act", multi=True)
                        b1_ = b1s[e % 2]
                        k.dma("pool", b1_[0:1, 0, :], b1g.h[l, e], [b1g], [b1_])
                        k.dma("pool", b1_[0:1, 1, :], b1l.h[l, e], [b1l], [b1_])
                        w2_ = w2s[e % 2]
                        b2_ = b2s[e % 2]
                        for c in range(8):
                            k.dma("pool", w2_[:, c, :], w2.h[l, e][c * 128:(c + 1) * 128, :], [w2], [w2_])
                        k.dma("pool", b2_[0:1, :], b2.h[l, e], [b2], [b2_])
                        for jt in range(8):
                            w1_ = w1s[nw % 3]
                            nw += 1
                            k.dma("pool", w1_[:, :, 0, :], w1g.h[l, e].rearrange("(c p) n -> p c n", p=128)[:, :, jt * 128:(jt + 1) * 128], [w1g], [w1_])
                            k.dma("pool", w1_[:, :, 1, :], w1l.h[l, e].rearrange("(c p) n -> p c n", p=128)[:, :, jt * 128:(jt + 1) * 128], [w1l], [w1_])
                            for hf in range(2):
                                bG = 2 + (ne_ % 2) * 2
                                bL = bG + 1
                                for (bk, wi_) in ((bG, 0), (bL, 1)):
                                    for c in range(8):
                                        k.mm(pb(bk, HS), w1_[:, c, wi_, :], xT_[:, c, hf * HS:(hf + 1) * HS], c == 0, False, [w1_, xT_], [PB[bk]])
                                    k.mm(pb(bk, HS), b1_[0:1, wi_, jt * 128:(jt + 1) * 128], ones_b[0:1, 0:HS], False, True, [b1_, ones_b], [PB[bk]])
                                g_, l_, s_ = eg[ne_ % 2], el[ne_ % 2], esg[ne_ % 2]
                                ne_ += 1
                                k.ts(g_[:, :], pb(bG, HS), 7.0, None, ALU.min, None, [PB[bG]], [g_])
                                k.act(s_[:, :], g_[:, :], AF.Sigmoid, [g_], [s_], scale=1.702)
                                k.ts(l_[:, :], pb(bL, HS), 7.0, -7.0, ALU.min, ALU.max, [PB[bL]], [l_])
                                k.stt(l_[:, :], l_[:, :], 1.0, g_[:, :], ALU.add, ALU.mult, [l_, g_], [l_])
                                k.tt(a_[:, jt, hf * HS:(hf + 1) * HS], l_[:, :], s_[:, :], ALU.mult, [l_, s_], [a_], multi=True)
                        for st in range(CAP // 128):
                            ys = yst[st % 2]
                            for hf in range(2):
                                bk = 6 + hf
                                for jt in range(8):
                                    k.mm(pb(bk), a_[:, jt, st * 128:(st + 1) * 128], w2_[:, jt, hf * 512:(hf + 1) * 512], jt == 0, False, [a_, w2_], [PB[bk]])
                                k.mm(pb(bk), ones_b[0:1, 0:128], b2_[0:1, hf * 512:(hf + 1) * 512], False, True, [ones_b, b2_], [PB[bk]])
                                k.cp(ys[:, hf * 512:(hf + 1) * 512], pb(bk), [PB[bk]], [ys], eng="act", multi=True)
                            k.dma("sp", yr_d[e * CAP + st * 128:e * CAP + (st + 1) * 128, :], ys[:, :], [ys], [yr_d])
                k.barrier()
                if stop == "E":
                    break

                with contextlib.ExitStack() as pz:
                    def psb(name, shape, dt=F32):
                        return Buf(pz.enter_context(nc.sbuf_tensor(name + "_%d" % l, list(shape), dt)))
                    ln2p = psb("ln2p", [128, 8, 2])
                    k.dma("sp", ln2p[:, :, :], ln2.h[l], [ln2], [ln2p])
                    gk8 = [psb("gk%d" % j, [128, D], BF16) for j in range(8)]
                    ytok = [psb("ytok%d" % j, [128, D]) for j in range(2)]
                    xr = [psb("xr2_%d" % j, [128, 8, 512]) for j in range(2)]
                    for g in range(NG):
                        x_ = xr[g % 2]
                        k.dma("sp", x_[:, :, :], xres.h.rearrange("(c p) s -> p c s", p=128)[:, :, g * 512:(g + 1) * 512], [xres], [x_])
                        for t4 in range(4):
                            tt_ = g * 4 + t4
                            y_ = ytok[tt_ % 2]
                            gk = gk8[(tt_ % 2) * 4:(tt_ % 2) * 4 + 4]
                            for kk in range(4):
                                k.dma("pool", gk[kk][:, :], yr_d[:, :], [yr_d, dest_all], [gk[kk]], multi=False,
                                      indirect=dict(in_offset=bass.IndirectOffsetOnAxis(ap=dest_all[:, tt_, kk:kk + 1], axis=0), out_offset=None))
                                if kk == 0:
                                    k.ts(y_[:, :], gk[kk][:, :], wts_all[:, tt_, 0:1], None, ALU.mult, None, [gk[kk], wts_all], [y_])
                                else:
                                    k.stt(y_[:, :], gk[kk][:, :], wts_all[:, tt_, kk:kk + 1], y_[:, :], ALU.mult, ALU.add, [gk[kk], wts_all, y_], [y_])
                            for hf in range(2):
                                bk = (tt_ % 2) * 2 + hf
                                for c in range(4):
                                    cc = hf * 4 + c
                                    k.tr(psum[:, bk, c * 128:(c + 1) * 128], y_[:, cc * 128:(cc + 1) * 128], ident_f[:, :], [y_, ident_f], [PB[bk]])
                                xv = x_[:, hf * 4:(hf + 1) * 4, t4 * 128:(t4 + 1) * 128]
                                k.stt(xv, xv, ALPHA, psum[:, bk, :].rearrange("p (c t) -> p c t", c=4), ALU.mult, ALU.add, [x_, PB[bk]], [x_], multi=True)
                        resid_ln(x_, ln2p, g, yT_out if last else xres)
                k.barrier()
        k.barrier()
    return nc, k, dbg


def _bf(x):
    return np.ascontiguousarray(x, dtype=np.float32)


def make_inputs(inp, b):
    f = lambda a: np.ascontiguousarray(np.asarray(a), dtype=np.float32)
    w_in = f(inp["w_in"])
    b_in = f(inp["b_in"])
    def swap_cols(w, c0, nh):
        out = []
        for h in range(nh):
            blk = w[..., c0 + h * 64:c0 + (h + 1) * 64]
            out.append(np.concatenate([blk[..., 8:16], blk[..., 0:8], blk[..., 16:64]], axis=-1))
        return np.concatenate(out, axis=-1)
    w_sw = np.concatenate([swap_cols(w_in, O_NQ, 4), swap_cols(w_in, O_NKC, 1), swap_cols(w_in, O_NKS, 1), swap_cols(w_in, O_NKW, 1)], axis=-1)
    b_sw = np.concatenate([swap_cols(b_in, O_NQ, 4), swap_cols(b_in, O_NKC, 1), swap_cols(b_in, O_NKS, 1), swap_cols(b_in, O_NKW, 1)], axis=-1)
    m = {}
    m["xT"] = np.ascontiguousarray(f(inp["x"])[b].T)
    m["pos"] = np.ascontiguousarray(np.asarray(inp["positions"])[b:b + 1].astype(np.int32))
    m["w_in"] = w_in
    m["b_in"] = b_in[:, None, :]
    m["w_sw"] = np.ascontiguousarray(w_sw)
    m["b_sw"] = np.ascontiguousarray(b_sw[:, None, :])
    m["pe_kT"] = np.ascontiguousarray(f(inp["nsa_pe_k"]).transpose(0, 2, 1))
    m["pe_vT"] = np.ascontiguousarray(f(inp["nsa_pe_v"]).transpose(0, 2, 1))
    m["cw1k"] = f(inp["nsa_cmp_w1_k"])
    m["cw2k"] = f(inp["nsa_cmp_w2_k"])
    m["cw1v"] = f(inp["nsa_cmp_w1_v"])
    m["cw2v"] = f(inp["nsa_cmp_w2_v"])
    m["g_lng"] = f(inp["gmlp_ln_g"])[:, None, :]
    m["g_lnb"] = f(inp["gmlp_ln_b"])[:, None, :]
    m["g_wsT"] = np.ascontiguousarray(f(inp["gmlp_w_s"]).transpose(0, 3, 1, 2))
    m["g_bs"] = f(inp["gmlp_b_s"]).reshape(L, 1, 512)
    m["c_wT"] = np.ascontiguousarray(f(inp["conv_w"])[:, :, 0, :].transpose(0, 2, 1))
    m["c_par"] = np.ascontiguousarray(np.stack([f(inp["conv_b"]), f(inp["conv_ln_g"]), f(inp["conv_ln_b"])], axis=-1))
    m["w_br"] = f(inp["w_br"])
    m["w_o"] = f(inp["w_o"])
    lay = lambda g_, b_: np.ascontiguousarray(np.stack([f(g_).reshape(L, 8, 128), f(b_).reshape(L, 8, 128)], axis=-1).transpose(0, 2, 1, 3))
    m["ln1"] = lay(inp["ln1_g"], inp["ln1_b"])
    m["ln2"] = lay(inp["ln2_g"], inp["ln2_b"])
    m["w_r"] = f(inp["w_router"])
    m["b_r"] = f(inp["b_router"])[:, None, :]
    w1 = f(inp["w_exp1"])
    m["w1g"] = np.ascontiguousarray(w1[..., 0::2])
    m["w1l"] = np.ascontiguousarray(w1[..., 1::2])
    b1 = f(inp["b_exp1"])
    m["b1g"] = np.ascontiguousarray(b1[..., 0::2])[:, :, None, :]
    m["b1l"] = np.ascontiguousarray(b1[..., 1::2])[:, :, None, :]
    m["w2"] = f(inp["w_exp2"])
    m["b2"] = f(inp["b_exp2"])[:, :, None, :]
    for n, v in host_consts().items():
        m["c_" + n] = v
    return m


_CACHE = {}


def kernel(**inputs):
    if "nc" not in _CACHE:
        _CACHE["nc"] = build()[0]
    nc = _CACHE["nc"]
    shared = make_inputs(inputs, 0)
    in_maps = []
    x = np.asarray(inputs["x"], dtype=np.float32)
    pos = np.asarray(inputs["positions"]).astype(np.int32)
    NCORES = 4
    for c in range(NCORES):
        b = c % 4
        m = dict(shared)
        m["xT"] = np.ascontiguousarray(x[b].T)
        m["pos"] = np.ascontiguousarray(pos[b:b + 1])
        in_maps.append(m)
    res = run_bass_kernel_spmd(nc, in_maps, core_ids=list(range(NCORES)))
    out = np.stack([np.ascontiguousarray(res.results[b]["yT"].T) for b in range(4)], axis=0)
    return out.astype(np.float32)
```
